# Optimizing a Trainium2 kernel written in Bass

```python
import math
import numpy as np
import jax
import jax.numpy as jnp
from jax import lax

D_MODEL = 1024
BATCH = 4
SEQ = 8192
DEPTH = 2

GRID_W = 64
CTX_LEN = 256
NORM_EPS = 1e-6
NEG_INF = -1e30

SSD_D_INNER = 1024
SSD_HEAD_DIM = 64
SSD_HEADS = SSD_D_INNER // SSD_HEAD_DIM
SSD_GROUPS = 2
SSD_STATE = 128
SSD_CONV = 5
SSD_CHUNK = 128
SSD_XBC = SSD_D_INNER + 2 * SSD_GROUPS * SSD_STATE

NA_HEADS = 8
NA_HEAD_DIM = 64
NA_WIDTH = NA_HEADS * NA_HEAD_DIM
NA_WIN_ROWS = 8
NA_WIN_COLS = 16
NA_QCOLS = 16
NA_KCOLS = NA_QCOLS + NA_WIN_COLS

CONF_WIDTH = 512
CONF_KERNEL = 31

N_BRANCH = 3

D_FF = 2816
N_EXPERTS = 8
TOP_K = 2
D_FF_EXPERT = 2816

IN_WIDTHS = (SSD_D_INNER, SSD_XBC, SSD_HEADS, SSD_HEADS, NA_WIDTH, NA_WIDTH, NA_WIDTH, 2 * CONF_WIDTH, N_BRANCH * D_MODEL)
IN_COLS = sum(IN_WIDTHS)

kernel_name = 'hybrid_ssd_natten_conformer_moe_dit'


def rms_norm(x, g):
    x32 = x.astype(jnp.float32)
    y = x32 * lax.rsqrt(jnp.mean(x32 * x32, axis=-1, keepdims=True) + NORM_EPS)
    return (y * g.astype(jnp.float32)).astype(x.dtype)


def layer_norm(x, g, b):
    x32 = x.astype(jnp.float32)
    xc = x32 - jnp.mean(x32, axis=-1, keepdims=True)
    var = jnp.mean(xc * xc, axis=-1, keepdims=True)
    return (xc * lax.rsqrt(var + NORM_EPS) * g.astype(jnp.float32) + b.astype(jnp.float32)).astype(x.dtype)


def modulate(x, shift, scale):
    return x * (1 + scale) + shift


def dw_conv(u, w, bias):
    k = w.shape[0]
    y = lax.conv_general_dilated(u, w[:, None, :].astype(u.dtype), window_strides=(1,),
                                 padding=[(k // 2, k // 2)], dimension_numbers=('NWC', 'WIO', 'NWC'),
                                 feature_group_count=u.shape[-1])
    return y + bias.astype(u.dtype)


def ssd_scan(xs, dt, a, bm, cm, h0):
    nb, seq_len, n_heads, hp = xs.shape
    n_groups, n_state = bm.shape[2], bm.shape[3]
    hg = n_heads // n_groups
    q = SSD_CHUNK
    nc = seq_len // q
    f32 = jnp.float32
    xd = (xs.astype(f32) * dt[..., None]).reshape(nb, nc, q, n_groups, hg, hp)
    bm = bm.astype(f32).reshape(nb, nc, q, n_groups, n_state)
    cm = cm.astype(f32).reshape(nb, nc, q, n_groups, n_state)
    cs = jnp.cumsum(dt.reshape(nb, nc, q, n_groups, hg) * a.reshape(n_groups, hg), axis=2)
    cs_t = jnp.moveaxis(cs, 2, -1)
    lower = jnp.tril(jnp.ones((q, q), dtype=bool))
    decay = jnp.exp(jnp.where(lower, cs_t[..., :, None] - cs_t[..., None, :], -jnp.inf))
    scores = jnp.einsum('bclgn,bcsgn->bcgls', cm, bm)
    y_diag = jnp.einsum('bcgls,bcgels,bcsgep->bclgep', scores, decay, xd)
    decay_end = jnp.exp(cs[:, :, -1:] - cs)
    states = jnp.einsum('bcsgn,bcsge,bcsgep->bcgepn', bm, decay_end, xd)
    chunk_decay = jnp.exp(cs[:, :, -1])

    def step(h, inp):
        s_c, d_c = inp
        return h * d_c[..., None, None] + s_c, h

    h_last, h_in = lax.scan(step, h0.reshape(nb, n_groups, hg, hp, n_state),
                            (jnp.moveaxis(states, 1, 0), jnp.moveaxis(chunk_decay, 1, 0)))
    h_in = jnp.moveaxis(h_in, 0, 1)
    y_off = jnp.einsum('bclgn,bcgepn,bclge->bclgep', cm, h_in, jnp.exp(cs))
    y = (y_diag + y_off).reshape(nb, seq_len, n_heads, hp)
    return y, h_last.reshape(nb, n_heads, hp, n_state)


def ssd_prepare(xbc, conv_w, conv_b):
    u = jax.nn.silu(dw_conv(xbc, conv_w, conv_b))
    nb, seq_len = u.shape[:2]
    xs, bm, cm = jnp.split(u, [SSD_D_INNER, SSD_D_INNER + SSD_GROUPS * SSD_STATE], axis=-1)
    return (xs.reshape(nb, seq_len, SSD_HEADS, SSD_HEAD_DIM),
            bm.reshape(nb, seq_len, SSD_GROUPS, SSD_STATE),
            cm.reshape(nb, seq_len, SSD_GROUPS, SSD_STATE))


def ssd_direction(xs, bm, cm, dt_raw, a_log, dt_bias, h0, reverse):
    if reverse:
        xs, bm, cm, dt_raw = (jnp.flip(t, axis=1) for t in (xs, bm, cm, dt_raw))
    dt = jax.nn.softplus(dt_raw.astype(jnp.float32) + dt_bias.astype(jnp.float32))
    a = -jnp.exp(a_log.astype(jnp.float32))
    y, h = ssd_scan(xs, dt, a, bm, cm, h0)
    if reverse:
        y = jnp.flip(y, axis=1)
    return y, h


def ssd_output(y_f, y_b, xs, z, d_skip, norm_g, w_out):
    nb, seq_len = z.shape[:2]
    y = y_f + y_b + d_skip.astype(jnp.float32)[:, None] * xs.astype(jnp.float32)
    y = y.reshape(nb, seq_len, SSD_D_INNER).astype(z.dtype) * jax.nn.silu(z)
    return rms_norm(y, norm_g) @ w_out


def na_latent(q, k, v, k_ctx, v_ctx, rpb):
    nb, seq_len = q.shape[:2]
    rows = seq_len // GRID_W
    wr = min(NA_WIN_ROWS, rows)
    n_cb = GRID_W // NA_QCOLS
    qg = q.reshape(nb, rows, n_cb, NA_QCOLS, NA_HEADS, NA_HEAD_DIM)
    kg = k.reshape(nb, rows, GRID_W, NA_HEADS, NA_HEAD_DIM)
    vg = v.reshape(nb, rows, GRID_W, NA_HEADS, NA_HEAD_DIM)
    qcol = jnp.arange(GRID_W).reshape(n_cb, NA_QCOLS)
    col_start = jnp.clip(qcol - NA_WIN_COLS // 2, 0, GRID_W - NA_WIN_COLS)
    kcol = (jnp.clip(jnp.arange(n_cb) * NA_QCOLS - NA_WIN_COLS // 2, 0, GRID_W - NA_KCOLS)[:, None]
            + jnp.arange(NA_KCOLS))
    rel = kcol[:, None, :] - col_start[:, :, None]
    col_mask = (rel >= 0) & (rel < NA_WIN_COLS)
    dcol_idx = jnp.clip(kcol[:, None, :] - qcol[:, :, None] + NA_WIN_COLS - 1, 0, 2 * NA_WIN_COLS - 2)
    row_start = jnp.clip(jnp.arange(rows) - wr // 2, 0, rows - wr)
    scale = NA_HEAD_DIM ** -0.5
    n_win = wr * NA_KCOLS

    def one_row(inp):
        q_r, r, rs = inp
        k_r = lax.dynamic_slice_in_dim(kg, rs, wr, axis=1)[:, :, kcol]
        v_r = lax.dynamic_slice_in_dim(vg, rs, wr, axis=1)[:, :, kcol]
        drow_idx = rs + jnp.arange(wr) - r + NA_WIN_ROWS - 1
        bias = rpb[:, drow_idx[None, None, :, None], dcol_idx[:, :, None, :]]
        s_win = jnp.einsum('bmqhd,bwmkhd->bhmqwk', q_r, k_r, preferred_element_type=jnp.float32) * scale
        s_win = jnp.where(col_mask[:, :, None, :], s_win + bias.astype(jnp.float32), NEG_INF)
        s_ctx = jnp.einsum('bmqhd,bjhd->bhmqj', q_r, k_ctx, preferred_element_type=jnp.float32) * scale
        s = jnp.concatenate([s_win.reshape(s_win.shape[:4] + (n_win,)), s_ctx], axis=-1)
        p = jax.nn.softmax(s, axis=-1).astype(v.dtype)
        p_win = p[..., :n_win].reshape(s_win.shape)
        p_ctx = p[..., n_win:]
        return (jnp.einsum('bhmqwk,bwmkhd->bmqhd', p_win, v_r)
                + jnp.einsum('bhmqj,bjhd->bmqhd', p_ctx, v_ctx))

    out = lax.map(one_row, (jnp.moveaxis(qg, 1, 0), jnp.arange(rows), row_start))
    return jnp.moveaxis(out, 0, 1).reshape(nb, seq_len, NA_WIDTH)


def na_context(q, k, v):
    s = jnp.einsum('bihd,bjhd->bhij', q, k, preferred_element_type=jnp.float32) * NA_HEAD_DIM ** -0.5
    p = jax.nn.softmax(s, axis=-1).astype(v.dtype)
    o = jnp.einsum('bhij,bjhd->bihd', p, v)
    return o.reshape(o.shape[0], o.shape[1], NA_WIDTH)


def conformer_branch(glu_in, conv_w, conv_b, ln_g, ln_b, w_out):
    a, g = jnp.split(glu_in, 2, axis=-1)
    u = dw_conv(a * jax.nn.sigmoid(g), conv_w, conv_b)
    return jax.nn.silu(layer_norm(u, ln_g, ln_b)) @ w_out


def merge_branches(gate_logits, branches):
    gates = jnp.split(jax.nn.sigmoid(gate_logits.astype(jnp.float32)).astype(gate_logits.dtype), N_BRANCH, axis=-1)
    out = gates[0] * branches[0]
    for g, y in zip(gates[1:], branches[1:]):
        out = out + g * y
    return out


def heads(t):
    return t.reshape(t.shape[0], t.shape[1], NA_HEADS, NA_HEAD_DIM)


def token_mixer(h_lat, h_ctx, lp, need_ctx):
    wb = jnp.split(lp['w_in'], np.cumsum(IN_WIDTHS)[:-1].tolist(), axis=1)
    w_z, w_xbc, w_dtf, w_dtb, w_q, w_k, w_v, w_glu, w_gate = wb
    xs_c, bm_c, cm_c = ssd_prepare(h_ctx @ w_xbc, lp['ssd_conv_w'], lp['ssd_conv_b'])
    xs_l, bm_l, cm_l = ssd_prepare(h_lat @ w_xbc, lp['ssd_conv_w'], lp['ssd_conv_b'])
    h0 = jnp.zeros((h_ctx.shape[0], SSD_HEADS, SSD_HEAD_DIM, SSD_STATE), jnp.float32)
    a_log, dt_bias = lp['ssd_a_log'], lp['ssd_dt_bias']
    y_cf, st_f = ssd_direction(xs_c, bm_c, cm_c, h_ctx @ w_dtf, a_log[0], dt_bias[0], h0, False)
    y_cb, st_b = ssd_direction(xs_c, bm_c, cm_c, h_ctx @ w_dtb, a_log[1], dt_bias[1], h0, True)
    y_lf, _ = ssd_direction(xs_l, bm_l, cm_l, h_lat @ w_dtf, a_log[0], dt_bias[0], st_f, False)
    y_lb, _ = ssd_direction(xs_l, bm_l, cm_l, h_lat @ w_dtb, a_log[1], dt_bias[1], st_b, True)
    ssd_l = ssd_output(y_lf, y_lb, xs_l, h_lat @ w_z, lp['ssd_d'], lp['ssd_norm'], lp['ssd_out'])
    k_c = heads(h_ctx @ w_k)
    v_c = heads(h_ctx @ w_v)
    na_l = na_latent(heads(h_lat @ w_q), heads(h_lat @ w_k), heads(h_lat @ w_v), k_c, v_c, lp['na_rpb']) @ lp['na_out']
    conf_l = conformer_branch(h_lat @ w_glu, lp['conf_conv_w'], lp['conf_conv_b'], lp['conf_ln_g'], lp['conf_ln_b'], lp['conf_out'])
    out_l = merge_branches(h_lat @ w_gate, (ssd_l, na_l, conf_l)) @ lp['w_o']
    if not need_ctx:
        return out_l, None
    ssd_c = ssd_output(y_cf, y_cb, xs_c, h_ctx @ w_z, lp['ssd_d'], lp['ssd_norm'], lp['ssd_out'])
    na_c = na_context(heads(h_ctx @ w_q), k_c, v_c) @ lp['na_out']
    conf_c = conformer_branch(h_ctx @ w_glu, lp['conf_conv_w'], lp['conf_conv_b'], lp['conf_ln_g'], lp['conf_ln_b'], lp['conf_out'])
    out_c = merge_branches(h_ctx @ w_gate, (ssd_c, na_c, conf_c)) @ lp['w_o']
    return out_l, out_c


def swiglu(h, w_gate, w_up, w_down):
    return (jax.nn.silu(h @ w_gate) * (h @ w_up)) @ w_down


def moe_swiglu(h, w_router, w_gate, w_up, w_down):
    logits = (h @ w_router).astype(jnp.float32)
    top_v, top_i = lax.top_k(logits, TOP_K)
    top_w = jax.nn.softmax(top_v, axis=-1)
    combine = jnp.sum(jax.nn.one_hot(top_i, N_EXPERTS, dtype=jnp.float32) * top_w[..., None], axis=-2).astype(h.dtype)
    out = combine[..., 0:1] * swiglu(h, w_gate[0], w_up[0], w_down[0])
    for e in range(1, N_EXPERTS):
        out = out + combine[..., e:e + 1] * swiglu(h, w_gate[e], w_up[e], w_down[e])
    return out


def setup_inputs(seed: int = 0) -> dict:
    key = jax.random.key(seed)
    keys = list(jax.random.split(key, 40))
    f32 = jnp.float32
    n_dense = (DEPTH + 1) // 2
    n_moe = DEPTH // 2

    def nrm(shape, scale=1.0):
        return jax.random.normal(keys.pop(), shape, f32) * scale

    def gain(shape):
        return 1.0 + nrm(shape, 0.05)

    def unif(shape, lo, hi):
        return jax.random.uniform(keys.pop(), shape, f32, lo, hi)

    dt0 = jnp.exp(unif((DEPTH, 2, SSD_HEADS), math.log(1e-3), math.log(1e-1)))
    return {
        'x': nrm((BATCH, SEQ, D_MODEL)),
        'c': nrm((BATCH, D_MODEL)),
        'ctx': nrm((BATCH, CTX_LEN, D_MODEL)),
        'c_ctx': nrm((D_MODEL,)),
        'ada_w': nrm((DEPTH, D_MODEL, 6 * D_MODEL), 0.5 * D_MODEL ** -0.5),
        'ada_b': nrm((DEPTH, 6 * D_MODEL), 0.02),
        'norm_mix': gain((DEPTH, D_MODEL)),
        'norm_ffn': gain((DEPTH, D_MODEL)),
        'w_in': nrm((DEPTH, D_MODEL, IN_COLS), D_MODEL ** -0.5),
        'ssd_conv_w': nrm((DEPTH, SSD_CONV, SSD_XBC), SSD_CONV ** -0.5),
        'ssd_conv_b': nrm((DEPTH, SSD_XBC), 0.02),
        'ssd_a_log': jnp.log(unif((DEPTH, 2, SSD_HEADS), 1.0, 16.0)),
        'ssd_dt_bias': dt0 + jnp.log(-jnp.expm1(-dt0)),
        'ssd_d': gain((DEPTH, SSD_HEADS)),
        'ssd_norm': gain((DEPTH, SSD_D_INNER)),
        'ssd_out': nrm((DEPTH, SSD_D_INNER, D_MODEL), SSD_D_INNER ** -0.5),
        'na_rpb': nrm((DEPTH, NA_HEADS, 2 * NA_WIN_ROWS - 1, 2 * NA_WIN_COLS - 1), 0.1),
        'na_out': nrm((DEPTH, NA_WIDTH, D_MODEL), NA_WIDTH ** -0.5),
        'conf_conv_w': nrm((DEPTH, CONF_KERNEL, CONF_WIDTH), CONF_KERNEL ** -0.5),
        'conf_conv_b': nrm((DEPTH, CONF_WIDTH), 0.02),
        'conf_ln_g': gain((DEPTH, CONF_WIDTH)),
        'conf_ln_b': nrm((DEPTH, CONF_WIDTH), 0.02),
        'conf_out': nrm((DEPTH, CONF_WIDTH, D_MODEL), CONF_WIDTH ** -0.5),
        'w_o': nrm((DEPTH, D_MODEL, D_MODEL), D_MODEL ** -0.5),
        'ffn_gate': nrm((n_dense, D_MODEL, D_FF), D_MODEL ** -0.5),
        'ffn_up': nrm((n_dense, D_MODEL, D_FF), D_MODEL ** -0.5),
        'ffn_down': nrm((n_dense, D_FF, D_MODEL), D_FF ** -0.5),
        'moe_router': nrm((n_moe, D_MODEL, N_EXPERTS), D_MODEL ** -0.5),
        'moe_gate': nrm((n_moe, N_EXPERTS, D_MODEL, D_FF_EXPERT), D_MODEL ** -0.5),
        'moe_up': nrm((n_moe, N_EXPERTS, D_MODEL, D_FF_EXPERT), D_MODEL ** -0.5),
        'moe_down': nrm((n_moe, N_EXPERTS, D_FF_EXPERT, D_MODEL), D_FF_EXPERT ** -0.5),
        'final_norm': gain((D_MODEL,)),
    }


def reference(x, c, ctx, c_ctx, ada_w, ada_b, norm_mix, norm_ffn, w_in, ssd_conv_w, ssd_conv_b,
              ssd_a_log, ssd_dt_bias, ssd_d, ssd_norm, ssd_out, na_rpb, na_out, conf_conv_w, conf_conv_b,
              conf_ln_g, conf_ln_b, conf_out, w_o, ffn_gate, ffn_up, ffn_down, moe_router, moe_gate,
              moe_up, moe_down, final_norm):
    s_c = jax.nn.silu(c)
    s_cc = jax.nn.silu(c_ctx)
    for layer in range(DEPTH):
        need_ctx = layer < DEPTH - 1
        mod_l = jnp.split((s_c @ ada_w[layer] + ada_b[layer])[:, None, :], 6, axis=-1)
        mod_c = jnp.split((s_cc @ ada_w[layer] + ada_b[layer])[None, None, :], 6, axis=-1)
        lp = {
            'w_in': w_in[layer], 'ssd_conv_w': ssd_conv_w[layer], 'ssd_conv_b': ssd_conv_b[layer],
            'ssd_a_log': ssd_a_log[layer], 'ssd_dt_bias': ssd_dt_bias[layer], 'ssd_d': ssd_d[layer],
            'ssd_norm': ssd_norm[layer], 'ssd_out': ssd_out[layer], 'na_rpb': na_rpb[layer],
            'na_out': na_out[layer], 'conf_conv_w': conf_conv_w[layer], 'conf_conv_b': conf_conv_b[layer],
            'conf_ln_g': conf_ln_g[layer], 'conf_ln_b': conf_ln_b[layer], 'conf_out': conf_out[layer],
            'w_o': w_o[layer],
        }
        h_l = modulate(rms_norm(x, norm_mix[layer]), mod_l[0], mod_l[1])
        h_c = modulate(rms_norm(ctx, norm_mix[layer]), mod_c[0], mod_c[1])
        y_l, y_c = token_mixer(h_l, h_c, lp, need_ctx)
        x = x + mod_l[2] * y_l
        h_l = modulate(rms_norm(x, norm_ffn[layer]), mod_l[3], mod_l[4])
        if need_ctx:
            ctx = ctx + mod_c[2] * y_c
            h_c = modulate(rms_norm(ctx, norm_ffn[layer]), mod_c[3], mod_c[4])
            h_all = jnp.concatenate([h_c, h_l], axis=1)
            n_ctx = ctx.shape[1]
        else:
            h_all = h_l
            n_ctx = 0
        i = layer // 2
        if layer % 2 == 0:
            f_all = swiglu(h_all, ffn_gate[i], ffn_up[i], ffn_down[i])
        else:
            f_all = moe_swiglu(h_all, moe_router[i], moe_gate[i], moe_up[i], moe_down[i])
        x = x + mod_l[5] * f_all[:, n_ctx:]
        if need_ctx:
            ctx = ctx + mod_c[5] * f_all[:, :n_ctx]
    return rms_norm(x, final_norm)
```

```python
import numpy as np
from contextlib import ExitStack
import concourse.bass as bass
import concourse.mybir as mybir
from concourse.bass_utils import run_bass_kernel_spmd

F32 = mybir.dt.float32
BF16 = mybir.dt.bfloat16
AF = mybir.ActivationFunctionType
ALU = mybir.AluOpType
AX = mybir.AxisListType

ENGS = ['pe', 'act', 'dve', 'pool', 'sp']
D = 1024
LCTX = 256
LLAT = 8192
T = LCTX + LLAT
NIN = 8224
DFF = 2816
EPS = 1e-6
NPC = 360
O_ADAB, O_GMIX, O_GFFN, O_SCW, O_SCB, O_CCW, O_CCB, O_LNG, O_LNB, O_SNORM, O_SD, O_ALOG, O_DTB = (
    0, 48, 56, 64, 124, 136, 260, 264, 268, 272, 280, 296, 328)
C_IDN, C_TRIF, C_TRIB, C_NEGF, C_NEGB, C_ONES = 0, 128, 256, 384, 512, 640
NCONST = 768


class Buf:
    __slots__ = ('name', 'w', 'r')

    def __init__(self, name):
        self.name = name
        self.w = None
        self.r = {}


class _Rec:
    def __getattr__(self, name):
        return lambda *a, **k: (name, a, k)


_REC = _Rec()


class Prog:
    def __init__(self, nc, stack, n_dma_sems=24):
        self.nc = nc
        self.q = {e: [] for e in ENGS}
        self.cnt = {e: 0 for e in ENGS}
        self.known = {e: {} for e in ENGS}
        self.esem = {}
        for e in ['pe', 'act', 'dve', 'pool']:
            self.esem[e] = stack.enter_context(nc.semaphore('c_' + e))
        self.dsem, self.dval, self.dnext = {}, {}, {}
        for e in ['sp', 'pool']:
            self.dsem[e] = [stack.enter_context(nc.semaphore('d_%s_%d' % (e, i))) for i in range(n_dma_sems)]
            self.dval[e] = [0] * n_dma_sems
            self.dnext[e] = 0
        self.dbufs = {}

    def buf(self, name='b'):
        return Buf(name)

    def D(self, name, idx=0):
        k = (name, idx)
        if k not in self.dbufs:
            self.dbufs[k] = Buf(str(k))
        return self.dbufs[k]

    def _collect(self, eng, reads, writes, extra=()):
        waits = {}

        def need(dep):
            if dep is None:
                return
            sem, val, deng = dep
            if eng == 'pe' and deng == 'pe':
                return
            k = id(sem)
            if k not in waits or waits[k][1] < val:
                waits[k] = (sem, val)

        for b in reads:
            need(b.w)
        for b in writes:
            need(b.w)
            for d in b.r.values():
                need(d)
        for d in extra:
            need(d)
        out = []
        kn = self.known[eng]
        for k, (sem, val) in waits.items():
            if kn.get(k, 0) >= val:
                continue
            kn[k] = val
            out.append((sem, val))
        return out

    def _commit(self, tok, reads, writes):
        for b in reads:
            b.r[id(tok[0])] = tok
        for b in writes:
            b.w = tok
            b.r = {}

    def op(self, eng, fn, reads=(), writes=()):
        waits = self._collect(eng, reads, writes)
        self.cnt[eng] += 1
        tok = (self.esem[eng], self.cnt[eng], eng)
        self._commit(tok, reads, writes)
        self.q[eng].append((waits, fn(_REC), self.esem[eng], 1))
        return tok

    def dma(self, eng, fn, reads=(), writes=()):
        i = self.dnext[eng]
        self.dnext[eng] = (i + 1) % len(self.dsem[eng])
        sem = self.dsem[eng][i]
        prev = self.dval[eng][i]
        extra = [(sem, prev, 'dma')] if prev > 0 else []
        waits = self._collect(eng, reads, writes, extra)
        self.dval[eng][i] = prev + 16
        tok = (sem, prev + 16, 'dma')
        self._commit(tok, reads, writes)
        self.q[eng].append((waits, fn(_REC), sem, 16))
        return tok

    def barrier(self):
        allw = []
        for e in ['sp', 'pool']:
            for s, v in zip(self.dsem[e], self.dval[e]):
                if v > 0:
                    allw.append((s, v))
        for e in ['pe', 'act', 'dve', 'pool']:
            if self.cnt[e] > 0:
                allw.append((self.esem[e], self.cnt[e]))
        for eng in ENGS:
            kn = self.known[eng]
            ws = []
            for s, v in allw:
                if kn.get(id(s), 0) < v:
                    kn[id(s)] = v
                    ws.append((s, v))
            if ws:
                self.q[eng].append((ws, None, None, 0))
        for b in self.dbufs.values():
            b.w = None
            b.r = {}

    def emit(self):
        nc = self.nc
        q = self.q

        def run(engine, items):
            for waits, fn, sem, inc in items:
                for s, v in waits:
                    engine.wait_ge(s, v)
                if fn is not None:
                    getattr(engine, fn[0])(*fn[1], **fn[2]).then_inc(sem, inc)

        with nc.Block() as block:
            @block.tensor
            def _(e):
                run(e, q['pe'])

            @block.scalar
            def _(e):
                run(e, q['act'])

            @block.vector
            def _(e):
                run(e, q['dve'])

            @block.gpsimd
            def _(e):
                run(e, q['pool'])

            @block.sync
            def _(e):
                run(e, q['sp'])


class TT:
    __slots__ = ('t', 'b')

    def __init__(self, t, b):
        self.t = t
        self.b = b


def kp(ap2d):
    return ap2d.rearrange("(k p) n -> p k n", p=128)


def token_tiles(with_ctx=True, size=512):
    tiles = []
    if with_ctx:
        for i in range(LCTX // min(size, LCTX)):
            tiles.append((i * min(size, LCTX), min(size, LCTX), 1))
    for i in range(LLAT // size):
        tiles.append((LCTX + size * i, size, 0))
    return tiles


class KB:
    def __init__(self, dbg=(), stop=None, nlayers=2, halfmode=True):
        self.halfmode = halfmode
        self.dbg = set(dbg)
        self.stop = stop
        self.nlayers = nlayers
        self.nc = bass.Bass("TRN2", target_bir_lowering=False)
        nc = self.nc
        self.inp = {}

        def I(name, shape):
            self.inp[name] = nc.dram_tensor(name, list(shape), F32, kind="ExternalInput").ap()

        I('xin', (D, T)); I('pcol', (2, 128, NPC)); I('gcol', (128, 24)); I('consts', (128, NCONST)); I('sel', (16, 2048))
        I('nabias', (2, 12, 128, 2560))
        I('ada_w', (2, D, 6 * D)); I('w_in', (2, D, NIN)); I('ssd_out', (2, D, D)); I('na_out', (2, 512, D))
        I('conf_out', (2, 512, D)); I('w_o', (2, D, D)); I('ffn_gate', (1, D, DFF)); I('ffn_up', (1, D, DFF))
        I('ffn_down', (1, DFF, D)); I('moe_router', (1, D, 8)); I('moe_gate', (1, 8, D, DFF)); I('moe_up', (1, 8, D, DFF))
        I('moe_down', (1, 8, DFF, D))
        self.out = nc.dram_tensor('outT', [D, LLAT // 2 if halfmode else LLAT], F32, kind="ExternalOutput").ap()
        self.scr = {}

        def S(name, shape, dt):
            kind = "ExternalOutput" if name in self.dbg else "Internal"
            self.scr[name] = nc.dram_tensor(name, list(shape), dt, kind=kind).ap()

        S('HT', (D, T), BF16); S('SZ', (T, D), BF16); S('XBCT', (1536, T), BF16); S('DTR', (T, 32), F32)
        S('QT', (512, T), BF16); S('KT', (512, T), BF16); S('VP', (T, 1024), BF16); S('UT', (512, T), BF16)
        S('GT', (3072, T), BF16); S('XS', (T, D), BF16); S('BMT', (T, 256), BF16); S('BCT', (512, T), BF16)
        S('YF', (T, D), F32); S('YNT', (D, T), BF16); S('NAT', (512, T), BF16); S('CFT', (512, T), BF16)
        S('X', (D, T), F32); S('H2T', (D, T), BF16); S('CMB', (8, T), F32)

    def sb(self, st, name, shape, dt):
        self._n += 1
        t = st.enter_context(self.nc.sbuf_tensor('%s_%d' % (name, self._n), list(shape), dt))
        return TT(t, self.P.buf(name))

    def ps(self, st, name, shape=None, dt=F32):
        self._n += 1
        shp = [128, 512] if dt == F32 else [128, 1024]
        t = st.enter_context(self.nc.psum_tensor('%s_%d' % (name, self._n), shp, dt))
        return TT(t, self.P.buf(name))

    def build(self):
        nc = self.nc
        with ExitStack() as st:
            self.P = Prog(nc, st)
            self._n = 0
            P = self.P
            self.cst = self.sb(st, 'cst', [128, NCONST], F32)
            self.selc = self.sb(st, 'selc', [16, 2048], F32)
            self.gcol = self.sb(st, 'gcol', [128, 24], F32)
            self.idb = self.sb(st, 'idb', [128, 128], BF16)
            self.onesb = self.sb(st, 'onesb', [128, 128], BF16)
            self.sv = self.sb(st, 'sv', [128, 8, 2], F32)
            P.dma('sp', lambda e: e.dma_start(out=self.cst.t[:], in_=self.inp['consts']), [], [self.cst.b])
            P.dma('sp', lambda e: e.dma_start(out=self.selc.t[:], in_=self.inp['sel']), [], [self.selc.b])
            P.dma('sp', lambda e: e.dma_start(out=self.gcol.t[:], in_=self.inp['gcol']), [], [self.gcol.b])
            P.op('dve', lambda e: e.tensor_copy(out=self.idb.t[:], in_=self.cst.t[:, C_IDN:C_IDN + 128]), [self.cst.b], [self.idb.b])
            P.op('dve', lambda e: e.tensor_copy(out=self.onesb.t[:], in_=self.cst.t[:, C_ONES:C_ONES + 128]), [self.cst.b], [self.onesb.b])
            P.op('act', lambda e: e.activation(out=self.sv.t[:].rearrange("p a b -> p (a b)"), in_=self.gcol.t[:, 8:24], func=AF.Silu),
                 [self.gcol.b], [self.sv.b])
            done = False
            for l in range(self.nlayers):
                with ExitStack() as lst:
                    done = self.layer(l, lst)
                P.barrier()
                if done:
                    break
            if not done:
                self.final_norm()
            P.barrier()
            P.emit()
        return nc

    def C(self, off):
        return self.cst.t[:, off:off + 128]

    def half(self, l):
        return self.halfmode and l == self.nlayers - 1

    def own_tiles(self, l, need_ctx, size=512):
        tl = token_tiles(need_ctx, size)
        if self.half(l):
            tl = [t for t in tl if t[0] < LCTX + LLAT // 2]
        return tl

    def layer(self, l, lst):
        P = self.P
        need_ctx = (l == 0)
        self.l = l
        self.pc = self.sb(lst, 'pc', [128, NPC], F32)
        P.dma('sp', lambda e: e.dma_start(out=self.pc.t[:], in_=self.inp['pcol'][l]), [], [self.pc.b])
        self.modc = self.sb(lst, 'modc', [128, 48, 2], F32)
        self.gm1 = self.sb(lst, 'gm1', [128, 8, 2], F32)
        self.gm2 = self.sb(lst, 'gm2', [128, 8, 2], F32)
        self.xsrc = self.inp['xin'] if l == 0 else self.scr['X']
        self.xsrc_name = 'xin' if l == 0 else 'X'
        phases = [('ada', self.ph_ada), ('norm1', self.ph_norm1), ('inproj', self.ph_inproj), ('sconv', self.ph_sconv),
                  ('ssd', self.ph_ssd), ('na', self.ph_na), ('conf', self.ph_conf), ('merge', self.ph_merge), ('ffn', self.ph_ffn)]
        for name, fn in phases:
            with ExitStack() as st:
                fn(st, l, need_ctx)
            P.barrier()
            if self.stop == (l, name):
                return True
        return False

    def ph_ada(self, st, l, need_ctx):
        P = self.P
        pc, modc = self.pc, self.modc
        aw = [self.sb(st, 'aw', [128, 8, 768], F32) for _ in range(2)]
        psm = self.ps(st, 'psm', [128, 96])
        src = self.inp['ada_w'][l]
        for s in range(8):
            a = aw[s % 2]
            P.dma('sp', lambda e, a=a, s=s: e.dma_start(out=a.t[:], in_=kp(src)[:, :, s * 768:(s + 1) * 768]), [], [a.b])
            for fi in range(6):
                fc = s * 6 + fi
                for kc in range(8):
                    P.op('pe', lambda e, a=a, fi=fi, fc=fc, kc=kc: e.matmul(psm.t[:, fc * 2:fc * 2 + 2], a.t[:, kc, fi * 128:(fi + 1) * 128],
                                                                           self.sv.t[:, kc, :], start=(kc == 0), stop=(kc == 7)),
                         [a.b, self.sv.b], [psm.b])
        P.op('dve', lambda e: e.tensor_tensor(out=modc.t[:], in0=psm.t[:, 0:96].rearrange("p (a b) -> p a b", b=2),
                                              in1=pc.t[:, O_ADAB:O_ADAB + 48].unsqueeze(2).to_broadcast([128, 48, 2]), op=ALU.add),
             [psm.b, pc.b], [modc.b])
        for (gm, goff, soff) in ((self.gm1, O_GMIX, 8), (self.gm2, O_GFFN, 32)):
            P.op('dve', lambda e, gm=gm, soff=soff: e.tensor_scalar(out=gm.t[:], in0=modc.t[:, soff:soff + 8, :], scalar1=1.0, scalar2=None, op0=ALU.add),
                 [modc.b], [gm.b])
            P.op('dve', lambda e, gm=gm, goff=goff: e.tensor_tensor(out=gm.t[:], in0=gm.t[:], in1=pc.t[:, goff:goff + 8].unsqueeze(2).to_broadcast([128, 8, 2]), op=ALU.mult),
                 [gm.b, pc.b], [gm.b])

    def rms_tile(self, xT, n, w, gm, shoff, psb, sq, rst, tmp, outs, extra_f32=None):
        P = self.P
        P.op('act', lambda e: e.activation(out=sq.t[:, :, 0:n], in_=xT.t[:, :, 0:n], func=AF.Square), [xT.b], [sq.b])
        for kc in range(8):
            P.op('pe', lambda e, kc=kc: e.matmul(psb.t[:, 0:n], self.onesb.t[:], sq.t[:, kc, 0:n], start=(kc == 0), stop=(kc == 7)),
                 [sq.b, self.onesb.b], [psb.b])
        P.op('act', lambda e: e.activation(out=rst.t[:, 0:n], in_=psb.t[:, 0:n], func=AF.Sqrt, scale=1.0 / D, bias=EPS), [psb.b], [rst.b])
        P.op('dve', lambda e: e.reciprocal(out=rst.t[:, 0:n], in_=rst.t[:, 0:n]), [rst.b], [rst.b])
        for c in range(8):
            if gm is None:
                P.op('dve', lambda e, c=c: e.scalar_tensor_tensor(out=outs.t[:, c, 0:n], in0=xT.t[:, c, 0:n], scalar=self.gcol.t[:, c:c + 1],
                                                                  in1=rst.t[:, 0:n], op0=ALU.mult, op1=ALU.mult), [xT.b, rst.b, self.gcol.b], [outs.b])
                continue
            P.op('dve', lambda e, c=c: e.scalar_tensor_tensor(out=tmp.t[:, 0:n], in0=xT.t[:, c, 0:n], scalar=gm.t[:, c, w:w + 1],
                                                              in1=rst.t[:, 0:n], op0=ALU.mult, op1=ALU.mult), [xT.b, rst.b, gm.b], [tmp.b])
            P.op('act', lambda e, c=c: e.activation(out=outs.t[:, c, 0:n], in_=tmp.t[:, 0:n], func=AF.Identity,
                                                    bias=self.modc.t[:, shoff + c, w:w + 1]), [tmp.b, self.modc.b], [outs.b])
            if extra_f32 is not None:
                P.op('act', lambda e, c=c: e.activation(out=extra_f32.t[:, c, 0:n], in_=tmp.t[:, 0:n], func=AF.Identity,
                                                        bias=self.modc.t[:, shoff + c, w:w + 1]), [tmp.b, self.modc.b], [extra_f32.b])

    def ph_norm1(self, st, l, need_ctx):
        P = self.P
        xT = [self.sb(st, 'xT', [128, 8, 512], F32) for _ in range(2)]
        hT = [self.sb(st, 'hT', [128, 8, 512], BF16) for _ in range(2)]
        sq = self.sb(st, 'sq', [128, 8, 512], BF16)
        rst = self.sb(st, 'rst', [128, 512], F32)
        tmp = self.sb(st, 'tmp', [128, 512], F32)
        psb = [self.ps(st, 'psb', [128, 512]) for _ in range(2)]
        tiles = token_tiles(True)
        src = kp(self.xsrc)
        dst = kp(self.scr['HT'])

        def load(i):
            t0, n, w = tiles[i]
            x = xT[i % 2]
            P.dma('sp', lambda e: e.dma_start(out=x.t[:, :, 0:n], in_=src[:, :, t0:t0 + n]), [P.D(self.xsrc_name, t0)], [x.b])

        load(0)
        for i, (t0, n, w) in enumerate(tiles):
            if i + 1 < len(tiles):
                load(i + 1)
            self.rms_tile(xT[i % 2], n, w, self.gm1, 0, psb[i % 2], sq, rst, tmp, hT[i % 2])
            h = hT[i % 2]
            P.dma('pool', lambda e, h=h, t0=t0, n=n: e.dma_start(out=dst[:, :, t0:t0 + n], in_=h.t[:, :, 0:n]), [h.b], [P.D('HT', t0)])

    def ph_inproj(self, st, l, need_ctx):
        P = self.P
        tiles = token_tiles(True)
        wsrc = kp(self.inp['w_in'][l])
        hsrc = kp(self.scr['HT'])
        W = [self.sb(st, 'W', [128, 8, 1536], BF16) for _ in range(2)]
        hT = [self.sb(st, 'hT', [128, 8, 512], BF16) for _ in range(3)]
        stg = [self.sb(st, 'stg', [128, 12, 512], BF16) for _ in range(2)]
        stgv = [self.sb(st, 'stgv', [128, 1024], BF16) for _ in range(2)]
        stgd = [self.sb(st, 'stgd', [128, 32], F32) for _ in range(2)]
        sg = [self.sb(st, 'sg', [128, 512], F32) for _ in range(2)]
        pss = [self.ps(st, 'pss', [128, 512]) for _ in range(6)]
        for s_ in stgv:
            P.op('pool', lambda e, s_=s_: e.memset(s_.t[:], 0.0), [], [s_.b])
        groups = [('z', 0, 1024, 'SZ'), ('fm', 1024, 1536, 'XBCT'), ('dt', 2560, 32, 'DTR'), ('fm', 2592, 512, 'QT'),
                  ('fm', 3104, 512, 'KT'), ('v', 3616, 512, 'VP'), ('glu', 4128, 1024, 'UT'),
                  ('sig', 5152, 1024, 'GT0'), ('sig', 6176, 1024, 'GT1'), ('sig', 7200, 1024, 'GT2')]
        cnt = {'ps': 0, 'stg': 0, 'h': 0, 'sg': 0}

        def nps():
            cnt['ps'] += 1
            return pss[cnt['ps'] % 6]

        def loadw(gi):
            kind, c0, ncols, dest = groups[gi]
            w = W[gi % 2]
            P.dma('pool', lambda e: e.dma_start(out=w.t[:, :, 0:ncols], in_=wsrc[:, :, c0:c0 + ncols]), [], [w.b])

        def loadh(ti, slot):
            t0, n, wi = tiles[ti]
            h = hT[slot % 3]
            P.dma('sp', lambda e: e.dma_start(out=h.t[:, :, 0:n], in_=hsrc[:, :, t0:t0 + n]), [P.D('HT', t0)], [h.b])

        loadw(0)
        nfull = len(tiles)
        if self.half(l):
            nfull = 1 + (LLAT // 2) // 512 + 1
        seq = [(gi, ti) for gi in range(len(groups)) for ti in range(len(tiles)) if ti < nfull or groups[gi][3] in ('XBCT', 'DTR')]
        loadh(seq[0][1], 0)
        for si, (gi, ti) in enumerate(seq):
            kind, c0, ncols, dest = groups[gi]
            t0, n, wi = tiles[ti]
            if ti == 0 and gi + 1 < len(groups):
                loadw(gi + 1)
            assert ti != 0 or True
            if si + 1 < len(seq):
                loadh(seq[si + 1][1], si + 1)
            w = W[gi % 2]
            h = hT[si % 3]
            if kind in ('fm', 'sig'):
                nch = ncols // 128
                cnt['stg'] += 1
                sg_ = stg[cnt['stg'] % 2]
                for j in range(nch):
                    p_ = nps()
                    for kc in range(8):
                        P.op('pe', lambda e, p_=p_, j=j, kc=kc: e.matmul(p_.t[:, 0:n], w.t[:, kc, j * 128:(j + 1) * 128], h.t[:, kc, 0:n],
                                                                        start=(kc == 0), stop=(kc == 7)), [w.b, h.b], [p_.b])
                    if kind == 'sig':
                        P.op('act', lambda e, p_=p_, j=j: e.activation(out=sg_.t[:, j, 0:n], in_=p_.t[:, 0:n], func=AF.Sigmoid), [p_.b], [sg_.b])
                    elif j % 2 == 0:
                        P.op('dve', lambda e, p_=p_, j=j: e.tensor_copy(out=sg_.t[:, j, 0:n], in_=p_.t[:, 0:n]), [p_.b], [sg_.b])
                    else:
                        P.op('act', lambda e, p_=p_, j=j: e.activation(out=sg_.t[:, j, 0:n], in_=p_.t[:, 0:n], func=AF.Copy), [p_.b], [sg_.b])
                if kind == 'sig':
                    gidx = int(dest[2])
                    dd = kp(self.scr['GT'])[:, gidx * 8:(gidx + 1) * 8, t0:t0 + n]
                    dn = 'GT'
                else:
                    dd = kp(self.scr[dest])[:, :, t0:t0 + n]
                    dn = dest
                P.dma('pool', lambda e, dd=dd, nch=nch: e.dma_start(out=dd, in_=sg_.t[:, 0:nch, 0:n]), [sg_.b], [P.D(dn, (t0, gi))])
            elif kind == 'glu':
                cnt['stg'] += 1
                sg_ = stg[cnt['stg'] % 2]
                for j in range(4):
                    pa, pg = nps(), nps()
                    for (p_, jj) in ((pa, j), (pg, j + 4)):
                        for kc in range(8):
                            P.op('pe', lambda e, p_=p_, jj=jj, kc=kc: e.matmul(p_.t[:, 0:n], w.t[:, kc, jj * 128:(jj + 1) * 128], h.t[:, kc, 0:n],
                                                                              start=(kc == 0), stop=(kc == 7)), [w.b, h.b], [p_.b])
                    cnt['sg'] += 1
                    s2 = sg[cnt['sg'] % 2]
                    P.op('act', lambda e, pg=pg, s2=s2: e.activation(out=s2.t[:, 0:n], in_=pg.t[:, 0:n], func=AF.Sigmoid), [pg.b], [s2.b])
                    P.op('dve', lambda e, pa=pa, s2=s2, j=j: e.tensor_tensor(out=sg_.t[:, j, 0:n], in0=pa.t[:, 0:n], in1=s2.t[:, 0:n], op=ALU.mult),
                         [pa.b, s2.b], [sg_.b])
                dd = kp(self.scr['UT'])[:, :, t0:t0 + n]
                P.dma('pool', lambda e, dd=dd: e.dma_start(out=dd, in_=sg_.t[:, 0:4, 0:n]), [sg_.b], [P.D('UT', t0)])
            else:
                for sub in range(n // 128):
                    tt = t0 + sub * 128
                    cnt['stg'] += 1
                    if kind == 'z':
                        sg_ = stg[cnt['stg'] % 2]
                        for half in range(2):
                            p_ = nps()
                            for kc in range(8):
                                P.op('pe', lambda e, p_=p_, half=half, kc=kc, sub=sub: e.matmul(
                                    p_.t[:, 0:512], h.t[:, kc, sub * 128:(sub + 1) * 128], w.t[:, kc, half * 512:(half + 1) * 512],
                                    start=(kc == 0), stop=(kc == 7)), [w.b, h.b], [p_.b])
                            P.op('act', lambda e, p_=p_, half=half: e.activation(out=sg_.t[:, half, :], in_=p_.t[:, 0:512], func=AF.Silu), [p_.b], [sg_.b])
                        P.dma('pool', lambda e, tt=tt: e.dma_start(out=self.scr['SZ'][tt:tt + 128, :].rearrange("p (a b) -> p a b", a=2), in_=sg_.t[:, 0:2, :]), [sg_.b], [P.D('SZ', tt)])
                    elif kind == 'v':
                        sv_ = stgv[cnt['stg'] % 2]
                        p_ = nps()
                        for kc in range(8):
                            P.op('pe', lambda e, p_=p_, kc=kc, sub=sub: e.matmul(p_.t[:, 0:512], h.t[:, kc, sub * 128:(sub + 1) * 128], w.t[:, kc, 0:512],
                                                                              start=(kc == 0), stop=(kc == 7)), [w.b, h.b], [p_.b])
                        pv = p_.t[:, 0:512].rearrange("p (a b d) -> p a b d", a=4, b=2)
                        ov = sv_.t[:].rearrange("p (a b c d) -> p a b c d", a=4, b=2, c=2)
                        P.op('dve', lambda e, pv=pv, ov=ov: e.tensor_copy(out=ov[:, :, 0, 0, :], in_=pv[:, :, 0, :]), [p_.b], [sv_.b])
                        P.op('act', lambda e, pv=pv, ov=ov: e.activation(out=ov[:, :, 1, 1, :], in_=pv[:, :, 1, :], func=AF.Copy), [p_.b], [sv_.b])
                        P.dma('pool', lambda e, tt=tt, sv_=sv_: e.dma_start(out=self.scr['VP'][tt:tt + 128, :], in_=sv_.t[:]), [sv_.b], [P.D('VP', tt)])
                    else:
                        sd_ = stgd[cnt['stg'] % 2]
                        p_ = nps()
                        for kc in range(8):
                            P.op('pe', lambda e, p_=p_, kc=kc, sub=sub: e.matmul(p_.t[:, 0:32], h.t[:, kc, sub * 128:(sub + 1) * 128], w.t[:, kc, 0:32],
                                                                              start=(kc == 0), stop=(kc == 7)), [w.b, h.b], [p_.b])
                        P.op('dve', lambda e, p_=p_, sd_=sd_: e.tensor_copy(out=sd_.t[:], in_=p_.t[:, 0:32]), [p_.b], [sd_.b])
                        P.dma('pool', lambda e, tt=tt, sd_=sd_: e.dma_start(out=self.scr['DTR'][tt:tt + 128, :], in_=sd_.t[:]), [sd_.b], [P.D('DTR', tt)])

    def make_diag(self, st, nch, ntap, woff):
        P = self.P
        dg = self.sb(st, 'dg', [128, nch, ntap, 128], BF16)
        for c in range(nch):
            for j in range(ntap):
                eng = 'dve' if (c * ntap + j) % 2 == 0 else 'pool'
                P.op(eng, lambda e, c=c, j=j: e.tensor_scalar(out=dg.t[:, c, j, :], in0=self.idb.t[:], scalar1=self.pc.t[:, woff + c * ntap + j:woff + c * ntap + j + 1],
                                                             scalar2=None, op0=ALU.mult), [self.idb.b, self.pc.b], [dg.b])
        return dg

    def seg_bounds(self, t0):
        return (0, LCTX) if t0 < LCTX else (LCTX, T)

    def load_halo(self, xb, srcname, nch, t0, n, halo):
        P = self.P
        lo, hi = self.seg_bounds(t0)
        a, b = max(lo, t0 - halo), min(hi, t0 + n + halo)
        if a != t0 - halo or b != t0 + n + halo:
            P.op('pool', lambda e: e.memset(xb.t[:, :, 0:n + 2 * halo], 0.0), [], [xb.b])
        o = a - (t0 - halo)
        src = kp(self.scr[srcname])[:, :, a:b]
        P.dma('sp', lambda e: e.dma_start(out=xb.t[:, :, o:o + (b - a)], in_=src), [P.D(srcname, 0)], [xb.b])

    def ph_sconv(self, st, l, need_ctx):
        P = self.P
        tiles = token_tiles(True)
        dg = self.make_diag(st, 12, 5, O_SCW)
        xb = [self.sb(st, 'xb', [128, 12, 516], BF16) for _ in range(2)]
        ux = [self.sb(st, 'ux', [128, 12, 512], BF16) for _ in range(2)]
        xtok = [self.sb(st, 'xtok', [128, 1280], BF16) for _ in range(2)]
        pcv = [self.ps(st, 'pcv', [128, 512]) for _ in range(3)]
        ptx = [self.ps(st, 'ptx', [128, 1024], BF16) for _ in range(2)]
        ptb = [self.ps(st, 'ptb', [128, 256], BF16) for _ in range(2)]
        self.load_halo(xb[0], 'XBCT', 12, tiles[0][0], tiles[0][1], 2)
        k = 0
        for i, (t0, n, w) in enumerate(tiles):
            if i + 1 < len(tiles):
                self.load_halo(xb[(i + 1) % 2], 'XBCT', 12, tiles[i + 1][0], tiles[i + 1][1], 2)
            x_, u_ = xb[i % 2], ux[i % 2]
            for c in range(12):
                p_ = pcv[c % 3]
                for j in range(5):
                    P.op('pe', lambda e, p_=p_, c=c, j=j: e.matmul(p_.t[:, 0:n], dg.t[:, c, j, :], x_.t[:, c, j:j + n], start=(j == 0), stop=(j == 4)),
                         [dg.b, x_.b], [p_.b])
                P.op('act', lambda e, p_=p_, c=c: e.activation(out=u_.t[:, c, 0:n], in_=p_.t[:, 0:n], func=AF.Silu, bias=self.pc.t[:, O_SCB + c:O_SCB + c + 1]),
                     [p_.b, self.pc.b], [u_.b])
            P.dma('pool', lambda e, u_=u_, t0=t0, n=n: e.dma_start(out=kp(self.scr['BCT'])[:, :, t0:t0 + n], in_=u_.t[:, 8:12, 0:n]), [u_.b], [P.D('BCT', t0)])
            for sub in range(n // 128):
                k += 1
                px, pb, xt = ptx[k % 2], ptb[k % 2], xtok[k % 2]
                for c in range(10):
                    o_ = px.t[:, c * 128:(c + 1) * 128] if c < 8 else pb.t[:, (c - 8) * 128:(c - 7) * 128]
                    P.op('pe', lambda e, o_=o_, c=c, sub=sub: e.transpose(o_, u_.t[:, c, sub * 128:(sub + 1) * 128], self.idb.t[:]),
                         [u_.b, self.idb.b], [px.b if c < 8 else pb.b])
                P.op('dve', lambda e, px=px, xt=xt: e.tensor_copy(out=xt.t[:, 0:1024], in_=px.t[:]), [px.b], [xt.b])
                P.op('act', lambda e, pb=pb, xt=xt: e.activation(out=xt.t[:, 1024:1280], in_=pb.t[:, 0:256], func=AF.Copy), [pb.b], [xt.b])
                tt = t0 + sub * 128
                P.dma('pool', lambda e, xt=xt, tt=tt: e.dma_start(out=self.scr['XS'][tt:tt + 128, :], in_=xt.t[:, 0:1024]), [xt.b], [P.D('XS', tt)])
                P.dma('pool', lambda e, xt=xt, tt=tt: e.dma_start(out=self.scr['BMT'][tt:tt + 128, :], in_=xt.t[:, 1024:1280]), [xt.b], [P.D('BMT', tt)])

    def ph_ssd(self, st, l, need_ctx):
        P = self.P
        pc = self.pc
        A = self.sb(st, 'A', [128, 32], F32)
        P.op('act', lambda e: e.activation(out=A.t[:], in_=pc.t[:, O_ALOG:O_ALOG + 32], func=AF.Exp), [pc.b], [A.b])
        P.op('dve', lambda e: e.tensor_scalar(out=A.t[:], in0=A.t[:], scalar1=-1.0, scalar2=None, op0=ALU.mult), [A.b], [A.b])
        DI = self.sb(st, 'DI', [128, 16, 128], BF16)
        for e_ in range(16):
            P.op('dve', lambda e, e_=e_: e.tensor_scalar(out=DI.t[:, e_, :], in0=self.idb.t[:], scalar1=pc.t[:, O_SD + e_:O_SD + e_ + 1], scalar2=None, op0=ALU.mult),
                 [self.idb.b, pc.b], [DI.b])
        gn = self.sb(st, 'gn', [128, 8], F32)
        P.op('dve', lambda e: e.tensor_copy(out=gn.t[:], in_=pc.t[:, O_SNORM:O_SNORM + 8]), [pc.b], [gn.b])
        hst = self.sb(st, 'hst', [128, 1024], F32)
        hbf = self.sb(st, 'hbf', [128, 1024], BF16)
        nb = 3
        xs = [self.sb(st, 'xs', [128, 1024], BF16) for _ in range(nb)]
        bmt = [self.sb(st, 'bmt', [128, 256], BF16) for _ in range(nb)]
        bct = [self.sb(st, 'bct', [128, 4, 128], BF16) for _ in range(nb)]
        dtr = [self.sb(st, 'dtr', [128, 32], F32) for _ in range(nb)]
        yfb = [self.sb(st, 'yfb', [128, 1024], F32) for _ in range(nb)]
        szb = [self.sb(st, 'szb', [128, 1024], BF16) for _ in range(nb)]
        sms = [{k: self.sb(st, k, [128, 16], F32) for k in ('t1', 'dt', 'dta', 'ncs', 'ecs', 'cd', 'w', 'wd')} for _ in range(2)]
        cs_sbs = [self.sb(st, 'cs_sb', [128, 32], F32) for _ in range(2)]
        csTs = [self.sb(st, 'csT', [16, 128], F32) for _ in range(2)]
        scTs = [self.sb(st, 'scT', [128, 2, 128], F32) for _ in range(2)]
        eL = [self.sb(st, 'eL', [128, 128], F32) for _ in range(4)]
        Malls = [self.sb(st, 'Mall', [128, 16, 128], BF16) for _ in range(2)]
        negb = self.sb(st, 'negb', [128, 2, 128], BF16)
        P.op('dve', lambda e: e.tensor_copy(out=negb.t[:, 0, :], in_=self.C(C_NEGF)), [self.cst.b], [negb.b])
        P.op('dve', lambda e: e.tensor_copy(out=negb.t[:, 1, :], in_=self.C(C_NEGB)), [self.cst.b], [negb.b])
        xw = self.sb(st, 'xw', [128, 1024], BF16)
        ytmp = self.sb(st, 'ytmp', [128, 1024], F32)
        ysq = self.sb(st, 'ysq', [128, 1024], F32)
        ynb = self.sb(st, 'ynb', [128, 1024], BF16)
        ynT = [self.sb(st, 'ynT', [128, 8, 128], BF16) for _ in range(2)]
        ssq = self.sb(st, 'ssq', [128, 1], F32)
        psA = self.ps(st, 'psA', [128, 512])
        psLs = [self.ps(st, 'psL', [128, 512]) for _ in range(2)]
        psYd = [self.ps(st, 'psYd', [128, 512]) for _ in range(2)]
        psYo = [self.ps(st, 'psYo', [128, 512]) for _ in range(2)]
        psT = self.ps(st, 'psT', [128, 1024], BF16)
        idn32, ones32 = self.C(C_IDN), self.C(C_ONES)
        cb = self.cst.b

        hf = self.half(l)
        nown = (LLAT // 2) // 128

        def chunk_list(direction):
            ctxc = [0, 128]
            lat = [LCTX + 128 * i for i in range(LLAT // 128)]
            if hf and direction == 0:
                lat = lat[:nown]
            return ctxc + lat if direction == 0 else ctxc[::-1] + lat[::-1]

        for d in range(2):
            tri = self.C(C_TRIF if d == 0 else C_TRIB)
            P.op('dve', lambda e: e.memset(hst.t[:], 0.0), [], [hst.b])
            P.op('act', lambda e: e.activation(out=hbf.t[:], in_=hst.t[:], func=AF.Copy), [hst.b], [hbf.b])
            chunks = chunk_list(d)

            def wanty(tc0):
                return ((tc0 >= LCTX) or need_ctx) and not (hf and tc0 >= LCTX + nown * 128)

            def load(ci):
                tc0 = chunks[ci]
                s_ = ci % nb
                P.dma('sp', lambda e: e.dma_start(out=xs[s_].t[:], in_=self.scr['XS'][tc0:tc0 + 128, :]), [P.D('XS', tc0)], [xs[s_].b])
                P.dma('sp', lambda e: e.dma_start(out=bmt[s_].t[:], in_=self.scr['BMT'][tc0:tc0 + 128, :]), [P.D('BMT', tc0)], [bmt[s_].b])
                P.dma('sp', lambda e: e.dma_start(out=bct[s_].t[:], in_=kp(self.scr['BCT'])[:, :, tc0:tc0 + 128]), [P.D('BCT', 0)], [bct[s_].b])
                P.dma('sp', lambda e: e.dma_start(out=dtr[s_].t[:], in_=self.scr['DTR'][tc0:tc0 + 128, :]), [P.D('DTR', tc0)], [dtr[s_].b])
                if d == 1 and wanty(tc0):
                    P.dma('sp', lambda e: e.dma_start(out=yfb[s_].t[:], in_=self.scr['YF'][tc0:tc0 + 128, :]), [P.D('YF', tc0)], [yfb[s_].b])
                    P.dma('sp', lambda e: e.dma_start(out=szb[s_].t[:], in_=self.scr['SZ'][tc0:tc0 + 128, :]), [P.D('SZ', tc0)], [szb[s_].b])

            def stageA(ci):
                tc0 = chunks[ci]
                s_ = ci % nb
                BC, DT = bct[s_], dtr[s_]
                sm, cs_sb, csT, scT, Mall = sms[ci % 2], cs_sbs[ci % 2], csTs[ci % 2], scTs[ci % 2], Malls[ci % 2]
                t1, dt, dta, ncs, ecs, cd, w_, wd = (sm[k] for k in ('t1', 'dt', 'dta', 'ncs', 'ecs', 'cd', 'w', 'wd'))
                P.op('dve', lambda e: e.tensor_tensor(out=t1.t[:], in0=DT.t[:, d * 16:(d + 1) * 16], in1=pc.t[:, O_DTB + d * 16:O_DTB + (d + 1) * 16], op=ALU.add),
                     [DT.b, pc.b], [t1.b])
                P.op('act', lambda e: e.activation(out=t1.t[:], in_=t1.t[:], func=AF.Exp), [t1.b], [t1.b])
                P.op('act', lambda e: e.activation(out=dt.t[:], in_=t1.t[:], func=AF.Ln, bias=1.0), [t1.b], [dt.b])
                P.op('dve', lambda e: e.tensor_tensor(out=dta.t[:], in0=dt.t[:], in1=A.t[:, d * 16:(d + 1) * 16], op=ALU.mult), [dt.b, A.b], [dta.b])
                P.op('pe', lambda e: e.matmul(psA.t[:, 0:16], tri, dta.t[:], start=True, stop=True), [cb, dta.b], [psA.b])
                P.op('pe', lambda e: e.matmul(psA.t[:, 16:32], ones32, dta.t[:], start=True, stop=True), [cb, dta.b], [psA.b])
                wy = wanty(tc0)
                if wy:
                    P.op('pe', lambda e: e.matmul(psA.t[0:16, 32:160], dta.t[:], tri, start=True, stop=True), [cb, dta.b], [psA.b])
                    for g in range(2):
                        P.op('pe', lambda e, g=g: e.matmul(psA.t[:, 160 + g * 128:160 + (g + 1) * 128], BC.t[:, g, :], BC.t[:, 2 + g, :], start=True, stop=True),
                             [BC.b], [psA.b])
                P.op('dve', lambda e: e.tensor_copy(out=cs_sb.t[:], in_=psA.t[:, 0:32]), [psA.b], [cs_sb.b])
                if wy:
                    P.op('act', lambda e: e.activation(out=csT.t[:], in_=psA.t[0:16, 32:160], func=AF.Copy), [psA.b], [csT.b])
                    P.op('act', lambda e: e.activation(out=scT.t[:].rearrange("p a b -> p (a b)"), in_=psA.t[:, 160:416], func=AF.Copy), [psA.b], [scT.b])
                P.op('dve', lambda e: e.tensor_scalar(out=ncs.t[:], in0=cs_sb.t[:, 0:16], scalar1=-1.0, scalar2=None, op0=ALU.mult), [cs_sb.b], [ncs.b])
                P.op('act', lambda e: e.activation(out=ecs.t[:], in_=cs_sb.t[:, 0:16], func=AF.Exp), [cs_sb.b], [ecs.b])
                P.op('act', lambda e: e.activation(out=cd.t[:], in_=cs_sb.t[:, 16:32], func=AF.Exp), [cs_sb.b], [cd.b])
                P.op('dve', lambda e: e.tensor_tensor(out=wd.t[:], in0=cs_sb.t[:, 16:32], in1=cs_sb.t[:, 0:16], op=ALU.subtract), [cs_sb.b], [wd.b])
                P.op('act', lambda e: e.activation(out=wd.t[:], in_=wd.t[:], func=AF.Exp), [wd.b], [wd.b])
                P.op('dve', lambda e: e.tensor_tensor(out=w_.t[:], in0=wd.t[:], in1=dt.t[:], op=ALU.mult), [wd.b, dt.b], [w_.b])
                if not wy:
                    return
                for e_ in range(16):
                    g = e_ // 8
                    sl = e_ % 4
                    psL = psLs[e_ % 2]
                    P.op('pe', lambda e, e_=e_: e.matmul(psL.t[:, 0:128], self.selc.t[0:16, e_ * 128:(e_ + 1) * 128], csT.t[:], start=True, stop=False),
                         [self.selc.b, csT.b], [psL.b])
                    P.op('pe', lambda e: e.matmul(psL.t[:, 0:128], self.idb.t[:], negb.t[:, d, :], start=False, stop=True), [self.idb.b, negb.b], [psL.b])
                    P.op('act', lambda e, e_=e_, sl=sl: e.activation(out=eL[sl].t[:], in_=psL.t[:, 0:128], func=AF.Exp, bias=ncs.t[:, e_:e_ + 1]),
                         [psL.b, ncs.b], [eL[sl].b])
                    P.op('dve', lambda e, e_=e_, sl=sl, g=g: e.scalar_tensor_tensor(out=Mall.t[:, e_, :], in0=eL[sl].t[:], scalar=dt.t[:, e_:e_ + 1], in1=scT.t[:, g, :],
                                                                               op0=ALU.mult, op1=ALU.mult), [eL[sl].b, dt.b, scT.b], [Mall.b])

            def stageB(ci):
                tc0 = chunks[ci]
                s_ = ci % nb
                X, BM, BC = xs[s_], bmt[s_], bct[s_]
                sm, Mall = sms[ci % 2], Malls[ci % 2]
                ecs, cd, w_ = sm['ecs'], sm['cd'], sm['w']
                if wanty(tc0):
                    for e_ in range(16):
                        g = e_ // 8
                        o_ = psYd[g].t[:, (e_ % 8) * 64:(e_ % 8 + 1) * 64]
                        P.op('pe', lambda e, o_=o_, e_=e_: e.matmul(o_, Mall.t[:, e_, :], X.t[:, e_ * 64:(e_ + 1) * 64], start=True, stop=(d == 1)),
                             [Mall.b, X.b], [psYd[g].b])
                        if d == 0:
                            P.op('pe', lambda e, o_=o_, e_=e_: e.matmul(o_, DI.t[:, e_, :], X.t[:, e_ * 64:(e_ + 1) * 64], start=False, stop=True),
                                 [DI.b, X.b], [psYd[g].b])
                    for g in range(2):
                        P.op('pe', lambda e, g=g: e.matmul(psYo[g].t[:], BC.t[:, 2 + g, :], hbf.t[:, g * 512:(g + 1) * 512], start=True, stop=True),
                             [BC.b, hbf.b], [psYo[g].b])
                    for g in range(2):
                        yv = ytmp.t[:, g * 512:(g + 1) * 512]
                        P.op('dve', lambda e, g=g, yv=yv: e.tensor_tensor(out=yv.rearrange("p (a b) -> p a b", b=64), in0=psYo[g].t[:].rearrange("p (a b) -> p a b", b=64),
                                                                         in1=ecs.t[:, g * 8:(g + 1) * 8].unsqueeze(2).to_broadcast([128, 8, 64]), op=ALU.mult),
                             [psYo[g].b, ecs.b], [ytmp.b])
                        P.op('dve', lambda e, g=g, yv=yv: e.tensor_tensor(out=yv, in0=yv, in1=psYd[g].t[:], op=ALU.add), [psYd[g].b, ytmp.b], [ytmp.b])
                    if d == 0:
                        P.dma('pool', lambda e: e.dma_start(out=self.scr['YF'][tc0:tc0 + 128, :], in_=ytmp.t[:]), [ytmp.b], [P.D('YF', tc0)])
                    else:
                        YFb, SZb = yfb[s_], szb[s_]
                        P.op('pool', lambda e: e.tensor_tensor(out=ytmp.t[:], in0=ytmp.t[:], in1=YFb.t[:], op=ALU.add), [ytmp.b, YFb.b], [ytmp.b])
                        P.op('pool', lambda e: e.tensor_tensor(out=ytmp.t[:], in0=ytmp.t[:], in1=SZb.t[:], op=ALU.mult), [ytmp.b, SZb.b], [ytmp.b])
                        P.op('pool', lambda e: e.tensor_tensor(out=ysq.t[:], in0=ytmp.t[:], in1=ytmp.t[:], op=ALU.mult), [ytmp.b], [ysq.b])
                        P.op('dve', lambda e: e.reduce_sum(out=ssq.t[:], in_=ysq.t[:], axis=AX.X), [ysq.b], [ssq.b])
                        P.op('act', lambda e: e.activation(out=ssq.t[:], in_=ssq.t[:], func=AF.Sqrt, scale=1.0 / D, bias=EPS), [ssq.b], [ssq.b])
                        P.op('dve', lambda e: e.reciprocal(out=ssq.t[:], in_=ssq.t[:]), [ssq.b], [ssq.b])
                        P.op('act', lambda e: e.activation(out=ynb.t[:], in_=ytmp.t[:], func=AF.Identity, scale=ssq.t[:, 0:1]), [ytmp.b, ssq.b], [ynb.b])
                        for c in range(8):
                            P.op('pe', lambda e, c=c: e.transpose(psT.t[:, c * 128:(c + 1) * 128], ynb.t[:, c * 128:(c + 1) * 128], self.idb.t[:]),
                                 [ynb.b, self.idb.b], [psT.b])
                        yt_ = ynT[ci % 2]
                        P.op('dve', lambda e: e.tensor_tensor(out=yt_.t[:], in0=psT.t[:].rearrange("p (a b) -> p a b", b=128),
                                                             in1=gn.t[:].unsqueeze(2).to_broadcast([128, 8, 128]), op=ALU.mult), [psT.b, gn.b], [yt_.b])
                        P.dma('pool', lambda e: e.dma_start(out=kp(self.scr['YNT'])[:, :, tc0:tc0 + 128], in_=yt_.t[:]), [yt_.b], [P.D('YNT', tc0)])
                P.op('pool', lambda e: e.tensor_tensor(out=xw.t[:].rearrange("p (a b) -> p a b", b=64), in0=X.t[:].rearrange("p (a b) -> p a b", b=64),
                                                      in1=w_.t[:].unsqueeze(2).to_broadcast([128, 16, 64]), op=ALU.mult), [X.b, w_.b], [xw.b])
                for g in range(2):
                    P.op('pe', lambda e, g=g: e.matmul(psYo[g].t[:], BM.t[:, g * 128:(g + 1) * 128], xw.t[:, g * 512:(g + 1) * 512], start=True, stop=True),
                         [BM.b, xw.b], [psYo[g].b])
                P.op('pool', lambda e: e.tensor_tensor(out=hst.t[:].rearrange("p (a b) -> p a b", b=64), in0=hst.t[:].rearrange("p (a b) -> p a b", b=64),
                                                      in1=cd.t[:].unsqueeze(2).to_broadcast([128, 16, 64]), op=ALU.mult), [hst.b, cd.b], [hst.b])
                for g in range(2):
                    P.op('dve', lambda e, g=g: e.tensor_tensor(out=hst.t[:, g * 512:(g + 1) * 512], in0=hst.t[:, g * 512:(g + 1) * 512], in1=psYo[g].t[:], op=ALU.add),
                         [hst.b, psYo[g].b], [hst.b])
                P.op('act', lambda e: e.activation(out=hbf.t[:], in_=hst.t[:], func=AF.Copy), [hst.b], [hbf.b])

            n_ = len(chunks)
            load(0)
            if n_ > 1:
                load(1)
            stageA(0)
            for ci in range(n_):
                if ci + 2 < n_:
                    load(ci + 2)
                if ci + 1 < n_:
                    stageA(ci + 1)
                stageB(ci)

    def ph_na(self, st, l, need_ctx):
        P = self.P
        SC = 0.125
        kc_ = self.sb(st, 'kc', [128, 4, 256], BF16)
        vc_ = self.sb(st, 'vc', [128, 2, 1024], BF16)
        P.dma('sp', lambda e: e.dma_start(out=kc_.t[:], in_=kp(self.scr['KT'])[:, :, 0:LCTX]), [P.D('KT', 0)], [kc_.b])
        P.dma('sp', lambda e: e.dma_start(out=vc_.t[:], in_=self.scr['VP'][0:LCTX, :].rearrange("(j p) n -> p j n", p=128)), [P.D('VP', 0)], [vc_.b])
        oa = [self.sb(st, 'oa', [128, 128], BF16) for _ in range(2)]
        for par in range(2):
            P.op('dve', lambda e, par=par: e.memset(oa[par].t[:], 0.0), [], [oa[par].b])
            P.op('dve', lambda e, par=par: e.memset(oa[par].t[:, par * 64:(par + 1) * 64], 1.0), [oa[par].b], [oa[par].b])
        bias = self.sb(st, 'bias', [128, 8, 448], F32)
        P.op('dve', lambda e: e.memset(bias.t[:], 0.0), [], [bias.b])
        kw = [self.sb(st, 'kw', [128, 4, 640], BF16) for _ in range(2)]
        vw = [self.sb(st, 'vw', [128, 5, 1024], BF16) for _ in range(2)]
        qr = [self.sb(st, 'qr', [128, 4, 64], BF16) for _ in range(2)]
        e1 = [self.sb(st, 'e1', [128, 448], F32) for _ in range(2)]
        pw = [self.sb(st, 'pw', [128, 448], BF16) for _ in range(3)]
        rec = [self.sb(st, 'rec', [128, 64], F32) for _ in range(2)]
        nao = [self.sb(st, 'nao', [128, 4, 512], BF16) for _ in range(2)]
        psS = [self.ps(st, 'psS', [128, 512]) for _ in range(3)]
        psO = [self.ps(st, 'psO', [128, 64]) for _ in range(2)]
        psM = [self.ps(st, 'psM', [128, 64]) for _ in range(2)]
        rows = []
        if need_ctx:
            rows += [('ctx', i) for i in range(4)]
        rows += [('lat', r) for r in range(64 if self.half(l) else 128)]
        cur_var = [None]
        cnt = {'s': 0, 'p': 0, 'o': 0, 'e': 0}

        def load(ri):
            kind, r = rows[ri]
            s_ = ri % 2
            tq0 = r * 64 if kind == 'ctx' else LCTX + r * 64
            P.dma('sp', lambda e: e.dma_start(out=qr[s_].t[:], in_=kp(self.scr['QT'])[:, :, tq0:tq0 + 64]), [P.D('QT', 0)], [qr[s_].b])
            if kind == 'lat':
                rs = min(max(r - 4, 0), 118)
                k0 = LCTX + rs * 64
                P.dma('sp', lambda e: e.dma_start(out=kw[s_].t[:], in_=kp(self.scr['KT'])[:, :, k0:k0 + 640]), [P.D('KT', 0)], [kw[s_].b])
                P.dma('sp', lambda e: e.dma_start(out=vw[s_].t[:], in_=self.scr['VP'][k0:k0 + 640, :].rearrange("(j p) n -> p j n", p=128)), [P.D('VP', 0)], [vw[s_].b])

        units = []
        for ri, (kind, r) in enumerate(rows):
            for hp in range(4):
                for par in range(2):
                    units.append((ri, kind, r, hp, par))
        state = {}

        def stage1(ui):
            ri, kind, r, hp, par = units[ui]
            s_ = ri % 2
            Q, KW = qr[s_], kw[s_]
            if hp == 0 and par == 0:
                if kind == 'lat':
                    var = r if r < 5 else (5 if r <= 121 else 6 + (r - 122))
                    if cur_var[0] != var:
                        cur_var[0] = var
                        P.dma('sp', lambda e, var=var: e.dma_start(out=bias.t[:, :, 0:320], in_=self.inp['nabias'][l, var].rearrange("p (a b) -> p a b", a=8)), [], [bias.b])
            nwin = 5 if kind == 'lat' else 0
            h = hp * 2 + par
            pr = slice(par * 64, (par + 1) * 64)
            pS = psS[ui % 3]
            PW = pw[ui % 3]
            for j in range(nwin):
                P.op('pe', lambda e, j=j: e.matmul(pS.t[:, j * 64:(j + 1) * 64], KW.t[pr, hp, j * 128:(j + 1) * 128], Q.t[pr, hp, :], start=True, stop=True), [KW.b, Q.b], [pS.b])
            for j in range(2):
                P.op('pe', lambda e, j=j: e.matmul(pS.t[:, 320 + j * 64:320 + (j + 1) * 64], kc_.t[pr, hp, j * 128:(j + 1) * 128], Q.t[pr, hp, :], start=True, stop=True), [kc_.b, Q.b], [pS.b])
            if nwin:
                E1 = e1[ui % 2]
                P.op('dve', lambda e: e.scalar_tensor_tensor(out=E1.t[:], in0=pS.t[:, 0:448], scalar=SC, in1=bias.t[:, h, :], op0=ALU.mult, op1=ALU.add), [pS.b, bias.b], [E1.b])
                P.op('act', lambda e: e.activation(out=PW.t[:, 0:448], in_=E1.t[:], func=AF.Exp), [E1.b], [PW.b])
            else:
                P.op('act', lambda e: e.activation(out=PW.t[:, 320:448], in_=pS.t[:, 320:448], func=AF.Exp, scale=SC), [pS.b], [PW.b])

        def stage2(ui):
            ri, kind, r, hp, par = units[ui]
            s_ = ri % 2
            VW = vw[s_]
            nwin = 5 if kind == 'lat' else 0
            h = hp * 2 + par
            PW = pw[ui % 3]
            oi = (ui // 2) % 2
            pO, pM = psO[oi], psM[oi]
            nmm = 2 * (nwin + 2)
            for j in range(nwin + 2):
                if j < nwin:
                    vap, vb, pap = VW.t[:, j, h * 128:(h + 1) * 128], VW.b, PW.t[:, j * 64:(j + 1) * 64]
                else:
                    vap, vb = vc_.t[:, j - nwin, h * 128:(h + 1) * 128], vc_.b
                    pap = PW.t[:, 320 + (j - nwin) * 64:320 + (j - nwin + 1) * 64]
                mi = par * (nwin + 2) + j
                first, last = (mi == 0), (mi == nmm - 1)
                P.op('pe', lambda e, vap=vap, pap=pap, first=first, last=last: e.matmul(pO.t[:, 0:64], vap, pap, start=first, stop=last), [vb, PW.b], [pO.b])
                P.op('pe', lambda e, pap=pap, first=first, last=last: e.matmul(pM.t[:, 0:64], oa[par].t[:], pap, start=first, stop=last), [oa[par].b, PW.b], [pM.b])
            if par == 1:
                if kind == 'ctx':
                    blk, NO, tb, nn = r, nao[0], 0, 256
                else:
                    blk, NO, tb, nn = r % 8, nao[(1 + r // 8) % 2], LCTX + (r // 8) * 512, 512
                R_ = rec[oi]
                P.op('dve', lambda e: e.reciprocal(out=R_.t[:], in_=pM.t[:, 0:64]), [pM.b], [R_.b])
                P.op('dve', lambda e: e.tensor_tensor(out=NO.t[:, hp, blk * 64:(blk + 1) * 64], in0=pO.t[:, 0:64], in1=R_.t[:], op=ALU.mult), [pO.b, R_.b], [NO.b])
                last_of_tile = hp == 3 and ((kind == 'ctx' and r == 3) or (kind == 'lat' and blk == 7))
                if last_of_tile:
                    P.dma('pool', lambda e: e.dma_start(out=kp(self.scr['NAT'])[:, :, tb:tb + nn], in_=NO.t[:, :, 0:nn]), [NO.b], [P.D('NAT', tb)])

        load(0)
        for ui in range(len(units) + 1):
            if ui < len(units):
                stage1(ui)
            if ui >= 1:
                stage2(ui - 1)
            if ui < len(units) and units[ui][3] == 0 and units[ui][4] == 0 and units[ui][0] + 1 < len(rows):
                load(units[ui][0] + 1)

    def ph_conf(self, st, l, need_ctx):
        P = self.P
        pc = self.pc
        tiles = self.own_tiles(l, need_ctx)
        dg = self.make_diag(st, 4, 31, O_CCW)
        ub = [self.sb(st, 'ub', [128, 4, 542], BF16) for _ in range(2)]
        u32 = self.sb(st, 'u32', [128, 4, 512], F32)
        usq = self.sb(st, 'usq', [128, 4, 512], F32)
        mean = self.sb(st, 'mean', [128, 512], F32)
        rstd = self.sb(st, 'rstd', [128, 512], F32)
        tmp = self.sb(st, 'tmp', [128, 512], F32)
        cf = [self.sb(st, 'cf', [128, 4, 512], BF16) for _ in range(2)]
        pcv = [self.ps(st, 'pcv', [128, 512]) for _ in range(2)]
        psm = self.ps(st, 'psm', [128, 512])
        psq = self.ps(st, 'psq', [128, 512])
        ones32 = self.C(C_ONES)
        self.load_halo(ub[0], 'UT', 4, tiles[0][0], tiles[0][1], 15)
        for i, (t0, n, w) in enumerate(tiles):
            if i + 1 < len(tiles):
                self.load_halo(ub[(i + 1) % 2], 'UT', 4, tiles[i + 1][0], tiles[i + 1][1], 15)
            U = ub[i % 2]
            for c in range(4):
                p_ = pcv[c % 2]
                for j in range(31):
                    P.op('pe', lambda e, p_=p_, c=c, j=j: e.matmul(p_.t[:, 0:n], dg.t[:, c, j, :], U.t[:, c, j:j + n], start=(j == 0), stop=(j == 30)),
                         [dg.b, U.b], [p_.b])
                P.op('act', lambda e, p_=p_, c=c: e.activation(out=u32.t[:, c, 0:n], in_=p_.t[:, 0:n], func=AF.Identity, bias=pc.t[:, O_CCB + c:O_CCB + c + 1]),
                     [p_.b, pc.b], [u32.b])
            P.op('pool', lambda e: e.tensor_tensor(out=usq.t[:, :, 0:n], in0=u32.t[:, :, 0:n], in1=u32.t[:, :, 0:n], op=ALU.mult), [u32.b], [usq.b])
            for c in range(4):
                P.op('pe', lambda e, c=c: e.matmul(psm.t[:, 0:n], ones32, u32.t[:, c, 0:n], start=(c == 0), stop=(c == 3)), [self.cst.b, u32.b], [psm.b])
            for c in range(4):
                P.op('pe', lambda e, c=c: e.matmul(psq.t[:, 0:n], ones32, usq.t[:, c, 0:n], start=(c == 0), stop=(c == 3)), [self.cst.b, usq.b], [psq.b])
            P.op('dve', lambda e: e.tensor_scalar(out=mean.t[:, 0:n], in0=psm.t[:, 0:n], scalar1=1.0 / 512, scalar2=None, op0=ALU.mult), [psm.b], [mean.b])
            P.op('dve', lambda e: e.tensor_tensor(out=tmp.t[:, 0:n], in0=mean.t[:, 0:n], in1=mean.t[:, 0:n], op=ALU.mult), [mean.b], [tmp.b])
            P.op('dve', lambda e: e.scalar_tensor_tensor(out=rstd.t[:, 0:n], in0=psq.t[:, 0:n], scalar=1.0 / 512, in1=tmp.t[:, 0:n], op0=ALU.mult, op1=ALU.subtract),
                 [psq.b, tmp.b], [rstd.b])
            P.op('act', lambda e: e.activation(out=rstd.t[:, 0:n], in_=rstd.t[:, 0:n], func=AF.Sqrt, bias=EPS), [rstd.b], [rstd.b])
            P.op('dve', lambda e: e.reciprocal(out=rstd.t[:, 0:n], in_=rstd.t[:, 0:n]), [rstd.b], [rstd.b])
            CF = cf[i % 2]
            for c in range(4):
                eng = 'dve' if c % 2 == 0 else 'pool'
                P.op(eng, lambda e, c=c: e.tensor_tensor(out=u32.t[:, c, 0:n], in0=u32.t[:, c, 0:n], in1=mean.t[:, 0:n], op=ALU.subtract), [u32.b, mean.b], [u32.b])
                P.op(eng, lambda e, c=c: e.tensor_tensor(out=u32.t[:, c, 0:n], in0=u32.t[:, c, 0:n], in1=rstd.t[:, 0:n], op=ALU.mult), [u32.b, rstd.b], [u32.b])
                P.op('act', lambda e, c=c: e.activation(out=CF.t[:, c, 0:n], in_=u32.t[:, c, 0:n], func=AF.Silu, scale=pc.t[:, O_LNG + c:O_LNG + c + 1],
                                                        bias=pc.t[:, O_LNB + c:O_LNB + c + 1]), [u32.b, pc.b], [CF.b])
            P.dma('pool', lambda e, CF=CF, t0=t0, n=n: e.dma_start(out=kp(self.scr['CFT'])[:, :, t0:t0 + n], in_=CF.t[:, :, 0:n]), [CF.b], [P.D('CFT', t0)])

    def ph_merge(self, st, l, need_ctx):
        P = self.P
        moe = (l % 2 == 1)
        tiles = self.own_tiles(l, need_ctx, 256)
        Wso = self.sb(st, 'Wso', [128, 8, 1024], BF16)
        Wno = self.sb(st, 'Wno', [128, 4, 1024], BF16)
        Wco = self.sb(st, 'Wco', [128, 4, 1024], BF16)
        Wo = self.sb(st, 'Wo', [128, 8, 1024], BF16)
        for (wt, nm) in ((Wso, 'ssd_out'), (Wno, 'na_out'), (Wco, 'conf_out'), (Wo, 'w_o')):
            P.dma('pool', lambda e, wt=wt, nm=nm: e.dma_start(out=wt.t[:], in_=kp(self.inp[nm][l])), [], [wt.b])
        yn = [self.sb(st, 'yn', [128, 8, 256], BF16) for _ in range(2)]
        na = [self.sb(st, 'na', [128, 4, 256], BF16) for _ in range(2)]
        cf = [self.sb(st, 'cf', [128, 4, 256], BF16) for _ in range(2)]
        gt = [self.sb(st, 'gt', [128, 24, 256], BF16) for _ in range(2)]
        xT = [self.sb(st, 'xT', [128, 8, 256], F32) for _ in range(2)]
        mg = self.sb(st, 'mg', [128, 8, 256], BF16)
        m1s = [self.sb(st, 'm1', [128, 256], F32) for _ in range(2)]
        m2s = [self.sb(st, 'm2', [128, 256], F32) for _ in range(2)]
        sq = self.sb(st, 'sq', [128, 8, 256], BF16)
        rst = self.sb(st, 'rst', [128, 256], F32)
        tmp = self.sb(st, 'tmp', [128, 256], F32)
        h2 = [self.sb(st, 'h2', [128, 8, 256], BF16) for _ in range(2)]
        pA = [self.ps(st, 'pA', [128, 256]) for _ in range(2)]
        pBs = [self.ps(st, 'pB', [128, 256]) for _ in range(2)]
        pCs = [self.ps(st, 'pC', [128, 256]) for _ in range(2)]
        pW = [self.ps(st, 'pW', [128, 256]) for _ in range(1)]
        pN = pW[0]
        if moe:
            h2f = self.sb(st, 'h2f', [128, 8, 256], F32)
            wr = self.sb(st, 'wr', [128, 8, 8], F32)
            P.dma('sp', lambda e: e.dma_start(out=wr.t[:], in_=kp(self.inp['moe_router'][0])), [], [wr.b])
            lg = self.sb(st, 'lg', [128, 8], F32)
            r8 = {k: self.sb(st, k, [128, 8], F32) for k in ('eq', 'l2', 'sel', 'ex', 'cmb')}
            r1 = {k: self.sb(st, k, [128, 1], F32) for k in ('mx1', 'nm1', 'mx2', 'ss')}
            cT = [self.sb(st, 'cT', [8, 128], F32) for _ in range(2)]
            pR = self.ps(st, 'pR', [128, 8])
            pRT = pR
        else:
            h2f = None

        def load(i):
            t0, n, w = tiles[i]
            s_ = i % 2
            P.dma('sp', lambda e: e.dma_start(out=yn[s_].t[:, :, 0:n], in_=kp(self.scr['YNT'])[:, :, t0:t0 + n]), [P.D('YNT', 0)], [yn[s_].b])
            P.dma('sp', lambda e: e.dma_start(out=na[s_].t[:, :, 0:n], in_=kp(self.scr['NAT'])[:, :, t0:t0 + n]), [P.D('NAT', 0)], [na[s_].b])
            P.dma('sp', lambda e: e.dma_start(out=cf[s_].t[:, :, 0:n], in_=kp(self.scr['CFT'])[:, :, t0:t0 + n]), [P.D('CFT', 0)], [cf[s_].b])
            P.dma('sp', lambda e: e.dma_start(out=gt[s_].t[:, :, 0:n], in_=kp(self.scr['GT'])[:, :, t0:t0 + n]), [P.D('GT', 0)], [gt[s_].b])
            P.dma('sp', lambda e: e.dma_start(out=xT[s_].t[:, :, 0:n], in_=kp(self.xsrc)[:, :, t0:t0 + n]), [P.D(self.xsrc_name, t0)], [xT[s_].b])

        load(0)
        for i, (t0, n, w) in enumerate(tiles):
            if i + 1 < len(tiles):
                load(i + 1)
            s_ = i % 2
            YN, NA, CF, GT, XT = yn[s_], na[s_], cf[s_], gt[s_], xT[s_]
            for fo in range(8):
                a_ = pA[fo % 2]
                pB, pC = pBs[fo % 2], pCs[fo % 2]
                m1, m2 = m1s[fo % 2], m2s[fo % 2]
                fs = slice(fo * 128, (fo + 1) * 128)
                for kc in range(8):
                    P.op('pe', lambda e, a_=a_, kc=kc, fs=fs: e.matmul(a_.t[:, 0:n], Wso.t[:, kc, fs], YN.t[:, kc, 0:n], start=(kc == 0), stop=(kc == 7)), [Wso.b, YN.b], [a_.b])
                for kc in range(4):
                    P.op('pe', lambda e, kc=kc, fs=fs: e.matmul(pB.t[:, 0:n], Wno.t[:, kc, fs], NA.t[:, kc, 0:n], start=(kc == 0), stop=(kc == 3)), [Wno.b, NA.b], [pB.b])
                for kc in range(4):
                    P.op('pe', lambda e, kc=kc, fs=fs: e.matmul(pC.t[:, 0:n], Wco.t[:, kc, fs], CF.t[:, kc, 0:n], start=(kc == 0), stop=(kc == 3)), [Wco.b, CF.b], [pC.b])
                P.op('dve', lambda e, a_=a_, fo=fo: e.tensor_tensor(out=m1.t[:, 0:n], in0=a_.t[:, 0:n], in1=GT.t[:, fo, 0:n], op=ALU.mult), [a_.b, GT.b], [m1.b])
                P.op('dve', lambda e, fo=fo: e.tensor_tensor(out=m2.t[:, 0:n], in0=pB.t[:, 0:n], in1=GT.t[:, 8 + fo, 0:n], op=ALU.mult), [pB.b, GT.b], [m2.b])
                P.op('pool', lambda e: e.tensor_tensor(out=m1.t[:, 0:n], in0=m1.t[:, 0:n], in1=m2.t[:, 0:n], op=ALU.add), [m1.b, m2.b], [m1.b])
                P.op('dve', lambda e, fo=fo: e.tensor_tensor(out=m2.t[:, 0:n], in0=pC.t[:, 0:n], in1=GT.t[:, 16 + fo, 0:n], op=ALU.mult), [pC.b, GT.b], [m2.b])
                P.op('pool', lambda e, fo=fo: e.tensor_tensor(out=mg.t[:, fo, 0:n], in0=m1.t[:, 0:n], in1=m2.t[:, 0:n], op=ALU.add), [m1.b, m2.b], [mg.b])
            for fo in range(8):
                p_ = pW[0]
                fs = slice(fo * 128, (fo + 1) * 128)
                for kc in range(8):
                    P.op('pe', lambda e, p_=p_, kc=kc, fs=fs: e.matmul(p_.t[:, 0:n], Wo.t[:, kc, fs], mg.t[:, kc, 0:n], start=(kc == 0), stop=(kc == 7)), [Wo.b, mg.b], [p_.b])
                P.op('dve', lambda e, p_=p_, fo=fo: e.scalar_tensor_tensor(out=XT.t[:, fo, 0:n], in0=p_.t[:, 0:n], scalar=self.modc.t[:, 16 + fo, w:w + 1], in1=XT.t[:, fo, 0:n],
                                                                          op0=ALU.mult, op1=ALU.add), [p_.b, XT.b, self.modc.b], [XT.b])
            P.dma('pool', lambda e, XT=XT, t0=t0, n=n: e.dma_start(out=kp(self.scr['X'])[:, :, t0:t0 + n], in_=XT.t[:, :, 0:n]), [XT.b], [P.D('X', t0)])
            H2 = h2[i % 2]
            self.rms_tile(XT, n, w, self.gm2, 24, pN, sq, rst, tmp, H2, extra_f32=h2f)
            P.dma('pool', lambda e, H2=H2, t0=t0, n=n: e.dma_start(out=kp(self.scr['H2T'])[:, :, t0:t0 + n], in_=H2.t[:, :, 0:n]), [H2.b], [P.D('H2T', t0)])
            if moe:
                for sub in range(n // 128):
                    for kc in range(8):
                        P.op('pe', lambda e, kc=kc, sub=sub: e.matmul(pR.t[:, 0:8], h2f.t[:, kc, sub * 128:(sub + 1) * 128], wr.t[:, kc, :], start=(kc == 0), stop=(kc == 7)),
                             [h2f.b, wr.b], [pR.b])
                    P.op('dve', lambda e: e.tensor_copy(out=lg.t[:], in_=pR.t[:, 0:8]), [pR.b], [lg.b])
                    eq, l2, sel, ex, cmb = (r8[k] for k in ('eq', 'l2', 'sel', 'ex', 'cmb'))
                    mx1, nm1, mx2, ss = (r1[k] for k in ('mx1', 'nm1', 'mx2', 'ss'))
                    P.op('dve', lambda e: e.reduce_max(out=mx1.t[:], in_=lg.t[:], axis=AX.X), [lg.b], [mx1.b])
                    P.op('dve', lambda e: e.tensor_scalar(out=eq.t[:], in0=lg.t[:], scalar1=mx1.t[:, 0:1], scalar2=None, op0=ALU.is_equal), [lg.b, mx1.b], [eq.b])
                    P.op('dve', lambda e: e.scalar_tensor_tensor(out=l2.t[:], in0=eq.t[:], scalar=-1e30, in1=lg.t[:], op0=ALU.mult, op1=ALU.add), [eq.b, lg.b], [l2.b])
                    P.op('dve', lambda e: e.reduce_max(out=mx2.t[:], in_=l2.t[:], axis=AX.X), [l2.b], [mx2.b])
                    P.op('dve', lambda e: e.tensor_scalar(out=sel.t[:], in0=lg.t[:], scalar1=mx2.t[:, 0:1], scalar2=None, op0=ALU.is_ge), [lg.b, mx2.b], [sel.b])
                    P.op('dve', lambda e: e.tensor_scalar(out=nm1.t[:], in0=mx1.t[:], scalar1=-1.0, scalar2=None, op0=ALU.mult), [mx1.b], [nm1.b])
                    P.op('act', lambda e: e.activation(out=ex.t[:], in_=lg.t[:], func=AF.Exp, bias=nm1.t[:, 0:1]), [lg.b, nm1.b], [ex.b])
                    P.op('dve', lambda e: e.tensor_tensor(out=ex.t[:], in0=ex.t[:], in1=sel.t[:], op=ALU.mult), [ex.b, sel.b], [ex.b])
                    P.op('dve', lambda e: e.reduce_sum(out=ss.t[:], in_=ex.t[:], axis=AX.X), [ex.b], [ss.b])
                    P.op('dve', lambda e: e.reciprocal(out=ss.t[:], in_=ss.t[:]), [ss.b], [ss.b])
                    P.op('dve', lambda e: e.tensor_scalar(out=cmb.t[:], in0=ex.t[:], scalar1=ss.t[:, 0:1], scalar2=None, op0=ALU.mult), [ex.b, ss.b], [cmb.b])
                    P.op('pe', lambda e: e.transpose(pRT.t[0:8, 128:256], cmb.t[:], self.C(C_IDN)), [cmb.b, self.cst.b], [pRT.b])
                    ct = cT[sub % 2]
                    P.op('act', lambda e, ct=ct: e.activation(out=ct.t[:], in_=pRT.t[0:8, 128:256], func=AF.Copy), [pRT.b], [ct.b])
                    tt = t0 + sub * 128
                    P.dma('pool', lambda e, ct=ct, tt=tt: e.dma_start(out=self.scr['CMB'][:, tt:tt + 128], in_=ct.t[:]), [ct.b], [P.D('CMB', tt)])
        self.xsrc = self.scr['X']
        self.xsrc_name = 'X'

    def ph_ffn(self, st, l, need_ctx):
        P = self.P
        moe = (l % 2 == 1)
        tiles = self.own_tiles(l, need_ctx)
        NH = DFF // 2
        Wg = self.sb(st, 'Wg', [128, 8, NH], BF16)
        Wu = self.sb(st, 'Wu', [128, 8, NH], BF16)
        Wd = self.sb(st, 'Wd', [128, 11, 1024], BF16)
        h2 = [self.sb(st, 'h2', [128, 8, 512], BF16) for _ in range(2)]
        xT = [self.sb(st, 'xT', [128, 8, 512], F32) for _ in range(2)]
        cw = [self.sb(st, 'cw', [128, 512], F32) for _ in range(2)]
        act = self.sb(st, 'act', [128, 11, 512], BF16)
        sg = [self.sb(st, 'sg', [128, 512], F32) for _ in range(2)]
        tmps = [self.sb(st, 'tmp', [128, 512], F32) for _ in range(2)]
        pG = [self.ps(st, 'pG', [128, 512]) for _ in range(2)]
        pU = [self.ps(st, 'pU', [128, 512]) for _ in range(2)]
        pD = [self.ps(st, 'pD', [128, 512]) for _ in range(2)]
        if moe:
            groups = [(ex, hf) for ex in range(8) for hf in range(2)]
        else:
            groups = [(None, hf) for hf in range(2)]
        for gi, (ex, hf) in enumerate(groups):
            if moe:
                gsrc, usrc, dsrc = self.inp['moe_gate'][0, ex], self.inp['moe_up'][0, ex], self.inp['moe_down'][0, ex]
            else:
                gsrc, usrc, dsrc = self.inp['ffn_gate'][0], self.inp['ffn_up'][0], self.inp['ffn_down'][0]
            P.dma('pool', lambda e, gsrc=gsrc, hf=hf: e.dma_start(out=Wg.t[:], in_=kp(gsrc)[:, :, hf * NH:(hf + 1) * NH]), [], [Wg.b])
            P.dma('pool', lambda e, usrc=usrc, hf=hf: e.dma_start(out=Wu.t[:], in_=kp(usrc)[:, :, hf * NH:(hf + 1) * NH]), [], [Wu.b])
            P.dma('pool', lambda e, dsrc=dsrc, hf=hf: e.dma_start(out=Wd.t[:], in_=kp(dsrc[hf * NH:(hf + 1) * NH, :])), [], [Wd.b])

            def load(i, gi=gi, ex=ex):
                t0, n, w = tiles[i]
                s_ = (gi * len(tiles) + i) % 2
                P.dma('sp', lambda e: e.dma_start(out=h2[s_].t[:, :, 0:n], in_=kp(self.scr['H2T'])[:, :, t0:t0 + n]), [P.D('H2T', 0)], [h2[s_].b])
                P.dma('sp', lambda e: e.dma_start(out=xT[s_].t[:, :, 0:n], in_=kp(self.scr['X'])[:, :, t0:t0 + n]), [P.D('X', t0)], [xT[s_].b])
                if moe:
                    P.dma('sp', lambda e: e.dma_start(out=cw[s_].t[:, 0:n], in_=self.scr['CMB'][ex:ex + 1, t0:t0 + n].partition_broadcast(128)), [P.D('CMB', 0)], [cw[s_].b])

            load(0)
            for i, (t0, n, w) in enumerate(tiles):
                if i + 1 < len(tiles):
                    load(i + 1)
                s_ = (gi * len(tiles) + i) % 2
                H2, XT, CW = h2[s_], xT[s_], cw[s_]
                for jf in range(11):
                    g_, u_ = pG[jf % 2], pU[jf % 2]
                    fs = slice(jf * 128, (jf + 1) * 128)
                    for kc in range(8):
                        P.op('pe', lambda e, g_=g_, kc=kc, fs=fs: e.matmul(g_.t[:, 0:n], Wg.t[:, kc, fs], H2.t[:, kc, 0:n], start=(kc == 0), stop=(kc == 7)), [Wg.b, H2.b], [g_.b])
                    for kc in range(8):
                        P.op('pe', lambda e, u_=u_, kc=kc, fs=fs: e.matmul(u_.t[:, 0:n], Wu.t[:, kc, fs], H2.t[:, kc, 0:n], start=(kc == 0), stop=(kc == 7)), [Wu.b, H2.b], [u_.b])
                    s2 = sg[jf % 2]
                    P.op('act', lambda e, g_=g_, s2=s2: e.activation(out=s2.t[:, 0:n], in_=g_.t[:, 0:n], func=AF.Silu), [g_.b], [s2.b])
                    P.op('dve', lambda e, u_=u_, s2=s2, jf=jf: e.tensor_tensor(out=act.t[:, jf, 0:n], in0=u_.t[:, 0:n], in1=s2.t[:, 0:n], op=ALU.mult), [u_.b, s2.b], [act.b])
                for fo in range(8):
                    d_ = pD[fo % 2]
                    fs = slice(fo * 128, (fo + 1) * 128)
                    for j in range(11):
                        P.op('pe', lambda e, d_=d_, j=j, fs=fs: e.matmul(d_.t[:, 0:n], Wd.t[:, j, fs], act.t[:, j, 0:n], start=(j == 0), stop=(j == 10)), [Wd.b, act.b], [d_.b])
                    gsc = self.modc.t[:, 40 + fo, w:w + 1]
                    if moe:
                        tm = tmps[fo % 2]
                        P.op('act', lambda e, d_=d_, tm=tm, gsc=gsc: e.activation(out=tm.t[:, 0:n], in_=d_.t[:, 0:n], func=AF.Identity, scale=gsc), [d_.b, self.modc.b], [tm.b])
                        P.op('pool', lambda e, tm=tm: e.tensor_tensor(out=tm.t[:, 0:n], in0=tm.t[:, 0:n], in1=CW.t[:, 0:n], op=ALU.mult), [tm.b, CW.b], [tm.b])
                        P.op('dve', lambda e, fo=fo, tm=tm: e.tensor_tensor(out=XT.t[:, fo, 0:n], in0=XT.t[:, fo, 0:n], in1=tm.t[:, 0:n], op=ALU.add), [tm.b, XT.b], [XT.b])
                    else:
                        P.op('dve', lambda e, d_=d_, fo=fo, gsc=gsc: e.scalar_tensor_tensor(out=XT.t[:, fo, 0:n], in0=d_.t[:, 0:n], scalar=gsc, in1=XT.t[:, fo, 0:n], op0=ALU.mult, op1=ALU.add),
                             [d_.b, XT.b, self.modc.b], [XT.b])
                P.dma('pool', lambda e, XT=XT, t0=t0, n=n: e.dma_start(out=kp(self.scr['X'])[:, :, t0:t0 + n], in_=XT.t[:, :, 0:n]), [XT.b], [P.D('X', t0)])

    def final_norm(self):
        P = self.P
        with ExitStack() as st:
            tiles = self.own_tiles(self.nlayers - 1, False)
            xT = [self.sb(st, 'xT', [128, 8, 512], F32) for _ in range(2)]
            oT = [self.sb(st, 'oT', [128, 8, 512], F32) for _ in range(2)]
            sq = self.sb(st, 'sq', [128, 8, 512], BF16)
            rst = self.sb(st, 'rst', [128, 512], F32)
            psb = [self.ps(st, 'psb', [128, 512]) for _ in range(2)]

            def load(i):
                t0, n, w = tiles[i]
                P.dma('sp', lambda e: e.dma_start(out=xT[i % 2].t[:], in_=kp(self.scr['X'])[:, :, t0:t0 + n]), [P.D('X', t0)], [xT[i % 2].b])

            load(0)
            for i, (t0, n, w) in enumerate(tiles):
                if i + 1 < len(tiles):
                    load(i + 1)
                self.rms_tile(xT[i % 2], n, 0, None, 0, psb[i % 2], sq, rst, None, oT[i % 2])
                o = oT[i % 2]
                P.dma('pool', lambda e, o=o, t0=t0, n=n: e.dma_start(out=kp(self.out)[:, :, t0 - LCTX:t0 - LCTX + n], in_=o.t[:]), [o.b], [P.D('out', t0)])
            P.barrier()


def _col(v, n):
    return np.ascontiguousarray(np.asarray(v, np.float32).reshape(n, 128).T)


NA_VAR_ROWS = [0, 1, 2, 3, 4, 64, 122, 123, 124, 125, 126, 127]


def _build_nabias(rpb, flip):
    out = np.full((len(NA_VAR_ROWS), 128, 8, 320), -1e30, np.float32)
    ql = np.arange(64)
    q_true = 63 - ql if flip else ql
    k_true = 63 - ql if flip else ql
    cs_true = np.clip(q_true - 8, 0, 48)
    validc = (k_true[:, None] >= cs_true[None, :]) & (k_true[:, None] < cs_true[None, :] + 16)
    dcol = np.clip(k_true[:, None] - q_true[None, :] + 15, 0, 30)
    for vi, rl in enumerate(NA_VAR_ROWS):
        rs10 = min(max(rl - 4, 0), 118)
        r_true = 127 - rl if flip else rl
        rs_true = min(max(r_true - 4, 0), 120)
        for w in range(10):
            krl = rs10 + w
            kr_true = 127 - krl if flip else krl
            if not (rs_true <= kr_true <= rs_true + 7):
                continue
            drow = kr_true - r_true + 7
            vals = rpb[:, drow][:, dcol]
            vals = np.where(validc[None], vals, np.float32(-1e30))
            j, par = w // 2, w % 2
            out[vi, par * 64:(par + 1) * 64, :, j * 64:(j + 1) * 64] = vals.transpose(1, 0, 2)
    return out.reshape(len(NA_VAR_ROWS), 128, 2560)


def _consts():
    c = np.zeros((128, NCONST), np.float32)
    s = np.arange(128)[:, None]
    l = np.arange(128)[None, :]
    c[:, C_IDN:C_IDN + 128] = (s == l)
    c[:, C_TRIF:C_TRIF + 128] = (s <= l)
    c[:, C_TRIB:C_TRIB + 128] = (s >= l)
    c[:, C_NEGF:C_NEGF + 128] = np.where(l >= s, 0.0, -1e30)
    c[:, C_NEGB:C_NEGB + 128] = np.where(s >= l, 0.0, -1e30)
    c[:, C_ONES:C_ONES + 128] = 1.0
    sel = np.zeros((16, 16, 128), np.float32)
    for e in range(16):
        sel[e, e, :] = 1.0
    return c, sel.reshape(16, 2048)


def _pcol(inp, l, flip):
    p = np.zeros((128, NPC), np.float32)
    p[:, O_ADAB:O_ADAB + 48] = _col(inp['ada_b'][l], 48)
    p[:, O_GMIX:O_GMIX + 8] = _col(inp['norm_mix'][l], 8)
    p[:, O_GFFN:O_GFFN + 8] = _col(inp['norm_ffn'][l], 8)
    scw = np.asarray(inp['ssd_conv_w'][l], np.float32)
    ccw = np.asarray(inp['conf_conv_w'][l], np.float32)
    alog = np.asarray(inp['ssd_a_log'][l], np.float32)
    dtb = np.asarray(inp['ssd_dt_bias'][l], np.float32)
    if flip:
        scw, ccw, alog, dtb = scw[::-1], ccw[::-1], alog[::-1], dtb[::-1]
    p[:, O_SCW:O_SCW + 60] = scw.reshape(5, 12, 128).transpose(2, 1, 0).reshape(128, 60)
    p[:, O_SCB:O_SCB + 12] = _col(inp['ssd_conv_b'][l], 12)
    p[:, O_CCW:O_CCW + 124] = ccw.reshape(31, 4, 128).transpose(2, 1, 0).reshape(128, 124)
    p[:, O_CCB:O_CCB + 4] = _col(inp['conf_conv_b'][l], 4)
    p[:, O_LNG:O_LNG + 4] = _col(inp['conf_ln_g'][l], 4)
    p[:, O_LNB:O_LNB + 4] = _col(inp['conf_ln_b'][l], 4)
    p[:, O_SNORM:O_SNORM + 8] = _col(inp['ssd_norm'][l], 8)
    p[:, O_SD:O_SD + 16] = np.asarray(inp['ssd_d'][l], np.float32)[None, :]
    p[:, O_ALOG:O_ALOG + 32] = alog.reshape(1, 32)
    p[:, O_DTB:O_DTB + 32] = dtb.reshape(1, 32)
    return p


_NC_CACHE = {}


def make_in_maps(inputs, halfmode=True):
    inp = {k: np.asarray(v) for k, v in inputs.items()}
    consts, sel = _consts()
    shared = {}
    for k in ('ada_w', 'ssd_out', 'na_out', 'conf_out', 'w_o', 'ffn_gate', 'ffn_up', 'ffn_down', 'moe_router', 'moe_gate', 'moe_up', 'moe_down'):
        shared[k] = np.ascontiguousarray(inp[k], dtype=np.float32)
    w_in = np.ascontiguousarray(inp['w_in'], dtype=np.float32)
    w_in_f = w_in.copy()
    w_in_f[:, :, 2560:2576] = w_in[:, :, 2576:2592]
    w_in_f[:, :, 2576:2592] = w_in[:, :, 2560:2576]
    per_flip = {}
    for flip in (False, True):
        per_flip[flip] = dict(
            pcol=np.stack([_pcol(inp, 0, flip), _pcol(inp, 1, flip)]),
            nabias=np.stack([_build_nabias(np.asarray(inp['na_rpb'][l], np.float32), flip) for l in range(2)]),
            w_in=w_in_f if flip else w_in)
    maps = []
    for core in range(8):
        b = core % 4
        flip = halfmode and core >= 4
        cx, xx = inp['ctx'][b], inp['x'][b]
        if flip:
            cx, xx = cx[::-1], xx[::-1]
        xin = np.ascontiguousarray(np.concatenate([cx.T, xx.T], axis=1), dtype=np.float32)
        gcol = np.zeros((128, 24), np.float32)
        gcol[:, 0:8] = _col(inp['final_norm'], 8)
        cv = np.stack([_col(inp['c'][b], 8), _col(inp['c_ctx'], 8)], axis=2)
        gcol[:, 8:24] = cv.reshape(128, 16)
        m = dict(shared)
        m.update(per_flip[flip])
        m.update(consts=consts, sel=sel, xin=xin, gcol=gcol)
        maps.append(m)
    return maps


def kernel(**inputs):
    if 'nc' not in _NC_CACHE:
        _NC_CACHE['nc'] = KB().build()
    nc = _NC_CACHE['nc']
    maps = make_in_maps(inputs)
    res = run_bass_kernel_spmd(nc, maps, core_ids=list(range(8)))
    out = np.empty((4, LLAT, D), np.float32)
    for b in range(4):
        out[b, :LLAT // 2] = res.results[b]['outT'].T
        out[b, LLAT // 2:] = res.results[b + 4]['outT'].T[::-1]
    return out
```

```python
import numpy as np
from contextlib import ExitStack
import concourse.bass as bass
import concourse.mybir as mybir
from concourse.bass_utils import run_bass_kernel_spmd

F32 = mybir.dt.float32
BF16 = mybir.dt.bfloat16
AF = mybir.ActivationFunctionType
ALU = mybir.AluOpType
AX = mybir.AxisListType

ENGS = ['pe', 'act', 'dve', 'pool', 'sp']
D = 1024
LCTX = 256
LLAT = 8192
T = LCTX + LLAT
NIN = 8224
DFF = 2816
EPS = 1e-6
NPC = 360
O_ADAB, O_GMIX, O_GFFN, O_SCW, O_SCB, O_CCW, O_CCB, O_LNG, O_LNB, O_SNORM, O_SD, O_ALOG, O_DTB = (
    0, 48, 56, 64, 124, 136, 260, 264, 268, 272, 280, 296, 328)
C_IDN, C_TRIF, C_TRIB, C_NEGF, C_NEGB, C_ONES = 0, 128, 256, 384, 512, 640
NCONST = 768


class Buf:
    __slots__ = ('name', 'w', 'r')

    def __init__(self, name):
        self.name = name
        self.w = None
        self.r = {}


class _Rec:
    def __getattr__(self, name):
        return lambda *a, **k: (name, a, k)


_REC = _Rec()


class Prog:
    def __init__(self, nc, stack, n_dma_sems=24):
        self.nc = nc
        self.q = {e: [] for e in ENGS}
        self.cnt = {e: 0 for e in ENGS}
        self.known = {e: {} for e in ENGS}
        self.esem = {}
        for e in ['pe', 'act', 'dve', 'pool']:
            self.esem[e] = stack.enter_context(nc.semaphore('c_' + e))
        self.dsem, self.dval, self.dnext = {}, {}, {}
        for e in ['sp', 'pool']:
            self.dsem[e] = [stack.enter_context(nc.semaphore('d_%s_%d' % (e, i))) for i in range(n_dma_sems)]
            self.dval[e] = [0] * n_dma_sems
            self.dnext[e] = 0
        self.dbufs = {}

    def buf(self, name='b'):
        return Buf(name)

    def D(self, name, idx=0):
        k = (name, idx)
        if k not in self.dbufs:
            self.dbufs[k] = Buf(str(k))
        return self.dbufs[k]

    def _collect(self, eng, reads, writes, extra=()):
        waits = {}

        def need(dep):
            if dep is None:
                return
            sem, val, deng = dep
            if eng == 'pe' and deng == 'pe':
                return
            k = id(sem)
            if k not in waits or waits[k][1] < val:
                waits[k] = (sem, val)

        for b in reads:
            need(b.w)
        for b in writes:
            need(b.w)
            for d in b.r.values():
                need(d)
        for d in extra:
            need(d)
        out = []
        kn = self.known[eng]
        for k, (sem, val) in waits.items():
            if kn.get(k, 0) >= val:
                continue
            kn[k] = val
            out.append((sem, val))
        return out

    def _commit(self, tok, reads, writes):
        for b in reads:
            b.r[id(tok[0])] = tok
        for b in writes:
            b.w = tok
            b.r = {}

    def op(self, eng, fn, reads=(), writes=()):
        waits = self._collect(eng, reads, writes)
        self.cnt[eng] += 1
        tok = (self.esem[eng], self.cnt[eng], eng)
        self._commit(tok, reads, writes)
        self.q[eng].append((waits, fn(_REC), self.esem[eng], 1))
        return tok

    def dma(self, eng, fn, reads=(), writes=()):
        i = self.dnext[eng]
        self.dnext[eng] = (i + 1) % len(self.dsem[eng])
        sem = self.dsem[eng][i]
        prev = self.dval[eng][i]
        extra = [(sem, prev, 'dma')] if prev > 0 else []
        waits = self._collect(eng, reads, writes, extra)
        self.dval[eng][i] = prev + 16
        tok = (sem, prev + 16, 'dma')
        self._commit(tok, reads, writes)
        self.q[eng].append((waits, fn(_REC), sem, 16))
        return tok

    def barrier(self):
        allw = []
        for e in ['sp', 'pool']:
            for s, v in zip(self.dsem[e], self.dval[e]):
                if v > 0:
                    allw.append((s, v))
        for e in ['pe', 'act', 'dve', 'pool']:
            if self.cnt[e] > 0:
                allw.append((self.esem[e], self.cnt[e]))
        for eng in ENGS:
            kn = self.known[eng]
            ws = []
            for s, v in allw:
                if kn.get(id(s), 0) < v:
                    kn[id(s)] = v
                    ws.append((s, v))
            if ws:
                self.q[eng].append((ws, None, None, 0))
        for b in self.dbufs.values():
            b.w = None
            b.r = {}

    def emit(self):
        nc = self.nc
        q = self.q

        def run(engine, items):
            for waits, fn, sem, inc in items:
                for s, v in waits:
                    engine.wait_ge(s, v)
                if fn is not None:
                    getattr(engine, fn[0])(*fn[1], **fn[2]).then_inc(sem, inc)

        with nc.Block() as block:
            @block.tensor
            def _(e):
                run(e, q['pe'])

            @block.scalar
            def _(e):
                run(e, q['act'])

            @block.vector
            def _(e):
                run(e, q['dve'])

            @block.gpsimd
            def _(e):
                run(e, q['pool'])

            @block.sync
            def _(e):
                run(e, q['sp'])


class TT:
    __slots__ = ('t', 'b')

    def __init__(self, t, b):
        self.t = t
        self.b = b


def kp(ap2d):
    return ap2d.rearrange("(k p) n -> p k n", p=128)


def token_tiles(with_ctx=True, size=512):
    tiles = []
    if with_ctx:
        for i in range(LCTX // min(size, LCTX)):
            tiles.append((i * min(size, LCTX), min(size, LCTX), 1))
    for i in range(LLAT // size):
        tiles.append((LCTX + size * i, size, 0))
    return tiles


class KB:
    def __init__(self, dbg=(), stop=None, nlayers=2, halfmode=True):
        self.halfmode = halfmode
        self.dbg = set(dbg)
        self.stop = stop
        self.nlayers = nlayers
        self.nc = bass.Bass("TRN2", target_bir_lowering=False)
        nc = self.nc
        self.inp = {}

        def I(name, shape):
            self.inp[name] = nc.dram_tensor(name, list(shape), F32, kind="ExternalInput").ap()

        I('xin', (D, T)); I('pcol', (2, 128, NPC)); I('gcol', (128, 24)); I('consts', (128, NCONST)); I('sel', (16, 2048))
        I('nabias', (2, 12, 128, 2560))
        I('ada_w', (2, D, 6 * D)); I('w_in', (2, D, NIN)); I('ssd_out', (2, D, D)); I('na_out', (2, 512, D))
        I('conf_out', (2, 512, D)); I('w_o', (2, D, D)); I('ffn_gate', (1, D, DFF)); I('ffn_up', (1, D, DFF))
        I('ffn_down', (1, DFF, D)); I('moe_router', (1, D, 8)); I('moe_gate', (1, 8, D, DFF)); I('moe_up', (1, 8, D, DFF))
        I('moe_down', (1, 8, DFF, D))
        self.out = nc.dram_tensor('outT', [D, LLAT // 2 if halfmode else LLAT], F32, kind="ExternalOutput").ap()
        self.scr = {}

        def S(name, shape, dt):
            kind = "ExternalOutput" if name in self.dbg else "Internal"
            self.scr[name] = nc.dram_tensor(name, list(shape), dt, kind=kind).ap()

        S('HT', (D, T), BF16); S('SZ', (T, D), BF16); S('XBCT', (1536, T), BF16); S('DTR', (T, 32), F32)
        S('QT', (512, T), BF16); S('KT', (512, T), BF16); S('VP', (T, 1024), BF16); S('UT', (512, T), BF16)
        S('GT', (3072, T), BF16); S('XS', (T, D), BF16); S('BMT', (T, 256), BF16); S('BCT', (512, T), BF16)
        S('YF', (T, D), F32); S('YNT', (D, T), BF16); S('NAT', (512, T), BF16); S('CFT', (512, T), BF16)
        S('X', (D, T), F32); S('H2T', (D, T), BF16); S('CMB', (8, T), F32)

    def sb(self, st, name, shape, dt):
        self._n += 1
        t = st.enter_context(self.nc.sbuf_tensor('%s_%d' % (name, self._n), list(shape), dt))
        return TT(t, self.P.buf(name))

    def ps(self, st, name, shape=None, dt=F32):
        self._n += 1
        shp = [128, 512] if dt == F32 else [128, 1024]
        t = st.enter_context(self.nc.psum_tensor('%s_%d' % (name, self._n), shp, dt))
        return TT(t, self.P.buf(name))

    def build(self):
        nc = self.nc
        with ExitStack() as st:
            self.P = Prog(nc, st)
            self._n = 0
            P = self.P
            self.cst = self.sb(st, 'cst', [128, NCONST], F32)
            self.selc = self.sb(st, 'selc', [16, 2048], F32)
            self.gcol = self.sb(st, 'gcol', [128, 24], F32)
            self.idb = self.sb(st, 'idb', [128, 128], BF16)
            self.onesb = self.sb(st, 'onesb', [128, 128], BF16)
            self.sv = self.sb(st, 'sv', [128, 8, 2], F32)
            P.dma('sp', lambda e: e.dma_start(out=self.cst.t[:], in_=self.inp['consts']), [], [self.cst.b])
            P.dma('sp', lambda e: e.dma_start(out=self.selc.t[:], in_=self.inp['sel']), [], [self.selc.b])
            P.dma('sp', lambda e: e.dma_start(out=self.gcol.t[:], in_=self.inp['gcol']), [], [self.gcol.b])
            P.op('dve', lambda e: e.tensor_copy(out=self.idb.t[:], in_=self.cst.t[:, C_IDN:C_IDN + 128]), [self.cst.b], [self.idb.b])
            P.op('dve', lambda e: e.tensor_copy(out=self.onesb.t[:], in_=self.cst.t[:, C_ONES:C_ONES + 128]), [self.cst.b], [self.onesb.b])
            P.op('act', lambda e: e.activation(out=self.sv.t[:].rearrange("p a b -> p (a b)"), in_=self.gcol.t[:, 8:24], func=AF.Silu),
                 [self.gcol.b], [self.sv.b])
            done = False
            for l in range(self.nlayers):
                with ExitStack() as lst:
                    done = self.layer(l, lst)
                P.barrier()
                if done:
                    break
            if not done:
                self.final_norm()
            P.barrier()
            P.emit()
        return nc

    def C(self, off):
        return self.cst.t[:, off:off + 128]

    def half(self, l):
        return self.halfmode and l == self.nlayers - 1

    def own_tiles(self, l, need_ctx, size=512):
        tl = token_tiles(need_ctx, size)
        if self.half(l):
            tl = [t for t in tl if t[0] < LCTX + LLAT // 2]
        return tl

    def layer(self, l, lst):
        P = self.P
        need_ctx = (l == 0)
        self.l = l
        self.pc = self.sb(lst, 'pc', [128, NPC], F32)
        P.dma('sp', lambda e: e.dma_start(out=self.pc.t[:], in_=self.inp['pcol'][l]), [], [self.pc.b])
        self.modc = self.sb(lst, 'modc', [128, 48, 2], F32)
        self.gm1 = self.sb(lst, 'gm1', [128, 8, 2], F32)
        self.gm2 = self.sb(lst, 'gm2', [128, 8, 2], F32)
        self.xsrc = self.inp['xin'] if l == 0 else self.scr['X']
        self.xsrc_name = 'xin' if l == 0 else 'X'
        phases = [('ada', self.ph_ada), ('norm1', self.ph_norm1), ('inproj', self.ph_inproj), ('sconv', self.ph_sconv),
                  ('ssd', self.ph_ssd), ('na', self.ph_na), ('conf', self.ph_conf), ('merge', self.ph_merge), ('ffn', self.ph_ffn)]
        for name, fn in phases:
            with ExitStack() as st:
                fn(st, l, need_ctx)
            P.barrier()
            if self.stop == (l, name):
                return True
        return False

    def ph_ada(self, st, l, need_ctx):
        P = self.P
        pc, modc = self.pc, self.modc
        aw = [self.sb(st, 'aw', [128, 8, 768], F32) for _ in range(2)]
        psm = self.ps(st, 'psm', [128, 96])
        src = self.inp['ada_w'][l]
        for s in range(8):
            a = aw[s % 2]
            P.dma('sp', lambda e, a=a, s=s: e.dma_start(out=a.t[:], in_=kp(src)[:, :, s * 768:(s + 1) * 768]), [], [a.b])
            for fi in range(6):
                fc = s * 6 + fi
                for kc in range(8):
                    P.op('pe', lambda e, a=a, fi=fi, fc=fc, kc=kc: e.matmul(psm.t[:, fc * 2:fc * 2 + 2], a.t[:, kc, fi * 128:(fi + 1) * 128],
                                                                           self.sv.t[:, kc, :], start=(kc == 0), stop=(kc == 7)),
                         [a.b, self.sv.b], [psm.b])
        P.op('dve', lambda e: e.tensor_tensor(out=modc.t[:], in0=psm.t[:, 0:96].rearrange("p (a b) -> p a b", b=2),
                                              in1=pc.t[:, O_ADAB:O_ADAB + 48].unsqueeze(2).to_broadcast([128, 48, 2]), op=ALU.add),
             [psm.b, pc.b], [modc.b])
        for (gm, goff, soff) in ((self.gm1, O_GMIX, 8), (self.gm2, O_GFFN, 32)):
            P.op('dve', lambda e, gm=gm, soff=soff: e.tensor_scalar(out=gm.t[:], in0=modc.t[:, soff:soff + 8, :], scalar1=1.0, scalar2=None, op0=ALU.add),
                 [modc.b], [gm.b])
            P.op('dve', lambda e, gm=gm, goff=goff: e.tensor_tensor(out=gm.t[:], in0=gm.t[:], in1=pc.t[:, goff:goff + 8].unsqueeze(2).to_broadcast([128, 8, 2]), op=ALU.mult),
                 [gm.b, pc.b], [gm.b])

    def rms_tile(self, xT, n, w, gm, shoff, psb, sq, rst, tmp, outs, extra_f32=None):
        P = self.P
        P.op('act', lambda e: e.activation(out=sq.t[:, :, 0:n], in_=xT.t[:, :, 0:n], func=AF.Square), [xT.b], [sq.b])
        for kc in range(8):
            P.op('pe', lambda e, kc=kc: e.matmul(psb.t[:, 0:n], self.onesb.t[:], sq.t[:, kc, 0:n], start=(kc == 0), stop=(kc == 7)),
                 [sq.b, self.onesb.b], [psb.b])
        P.op('act', lambda e: e.activation(out=rst.t[:, 0:n], in_=psb.t[:, 0:n], func=AF.Sqrt, scale=1.0 / D, bias=EPS), [psb.b], [rst.b])
        P.op('dve', lambda e: e.reciprocal(out=rst.t[:, 0:n], in_=rst.t[:, 0:n]), [rst.b], [rst.b])
        for c in range(8):
            if gm is None:
                P.op('dve', lambda e, c=c: e.scalar_tensor_tensor(out=outs.t[:, c, 0:n], in0=xT.t[:, c, 0:n], scalar=self.gcol.t[:, c:c + 1],
                                                                  in1=rst.t[:, 0:n], op0=ALU.mult, op1=ALU.mult), [xT.b, rst.b, self.gcol.b], [outs.b])
                continue
            tm = tmp[c % len(tmp)] if isinstance(tmp, list) else tmp
            P.op('dve', lambda e, c=c, tm=tm: e.scalar_tensor_tensor(out=tm.t[:, 0:n], in0=xT.t[:, c, 0:n], scalar=gm.t[:, c, w:w + 1],
                                                                     in1=rst.t[:, 0:n], op0=ALU.mult, op1=ALU.mult), [xT.b, rst.b, gm.b], [tm.b])
            P.op('act', lambda e, c=c, tm=tm: e.activation(out=outs.t[:, c, 0:n], in_=tm.t[:, 0:n], func=AF.Identity,
                                                           bias=self.modc.t[:, shoff + c, w:w + 1]), [tm.b, self.modc.b], [outs.b])
            if extra_f32 is not None:
                P.op('act', lambda e, c=c, tm=tm: e.activation(out=extra_f32.t[:, c, 0:n], in_=tm.t[:, 0:n], func=AF.Identity,
                                                               bias=self.modc.t[:, shoff + c, w:w + 1]), [tm.b, self.modc.b], [extra_f32.b])

    def ph_norm1(self, st, l, need_ctx):
        P = self.P
        xT = [self.sb(st, 'xT', [128, 8, 512], F32) for _ in range(2)]
        hT = [self.sb(st, 'hT', [128, 8, 512], BF16) for _ in range(2)]
        sqs = [self.sb(st, 'sq', [128, 8, 512], BF16) for _ in range(2)]
        rsts = [self.sb(st, 'rst', [128, 512], F32) for _ in range(2)]
        tmp = [self.sb(st, 'tmp', [128, 512], F32) for _ in range(2)]
        psb = [self.ps(st, 'psb', [128, 512]) for _ in range(2)]
        tiles = token_tiles(True)
        src = kp(self.xsrc)
        dst = kp(self.scr['HT'])

        def load(i):
            t0, n, w = tiles[i]
            x = xT[i % 2]
            P.dma('sp', lambda e: e.dma_start(out=x.t[:, :, 0:n], in_=src[:, :, t0:t0 + n]), [P.D(self.xsrc_name, t0)], [x.b])

        load(0)
        for i, (t0, n, w) in enumerate(tiles):
            if i + 1 < len(tiles):
                load(i + 1)
            self.rms_tile(xT[i % 2], n, w, self.gm1, 0, psb[i % 2], sqs[i % 2], rsts[i % 2], tmp, hT[i % 2])
            h = hT[i % 2]
            P.dma('pool', lambda e, h=h, t0=t0, n=n: e.dma_start(out=dst[:, :, t0:t0 + n], in_=h.t[:, :, 0:n]), [h.b], [P.D('HT', t0)])

    def ph_inproj(self, st, l, need_ctx):
        P = self.P
        tiles = token_tiles(True)
        wsrc = kp(self.inp['w_in'][l])
        hsrc = kp(self.scr['HT'])
        W = [self.sb(st, 'W', [128, 8, 1536], BF16) for _ in range(2)]
        hT = [self.sb(st, 'hT', [128, 8, 512], BF16) for _ in range(3)]
        stg = [self.sb(st, 'stg', [128, 12, 512], BF16) for _ in range(2)]
        stgv = [self.sb(st, 'stgv', [128, 1024], BF16) for _ in range(2)]
        stgd = [self.sb(st, 'stgd', [128, 32], F32) for _ in range(2)]
        sg = [self.sb(st, 'sg', [128, 512], F32) for _ in range(2)]
        pss = [self.ps(st, 'pss', [128, 512]) for _ in range(6)]
        for s_ in stgv:
            P.op('pool', lambda e, s_=s_: e.memset(s_.t[:], 0.0), [], [s_.b])
        groups = [('z', 0, 1024, 'SZ'), ('fm', 1024, 1536, 'XBCT'), ('dt', 2560, 32, 'DTR'), ('fm', 2592, 512, 'QT'),
                  ('fm', 3104, 512, 'KT'), ('v', 3616, 512, 'VP'), ('glu', 4128, 1024, 'UT'),
                  ('sig', 5152, 1024, 'GT0'), ('sig', 6176, 1024, 'GT1'), ('sig', 7200, 1024, 'GT2')]
        cnt = {'ps': 0, 'stg': 0, 'h': 0, 'sg': 0}

        def nps():
            cnt['ps'] += 1
            return pss[cnt['ps'] % 6]

        def loadw(gi):
            kind, c0, ncols, dest = groups[gi]
            w = W[gi % 2]
            P.dma('pool', lambda e: e.dma_start(out=w.t[:, :, 0:ncols], in_=wsrc[:, :, c0:c0 + ncols]), [], [w.b])

        def loadh(ti, slot):
            t0, n, wi = tiles[ti]
            h = hT[slot % 3]
            P.dma('sp', lambda e: e.dma_start(out=h.t[:, :, 0:n], in_=hsrc[:, :, t0:t0 + n]), [P.D('HT', t0)], [h.b])

        loadw(0)
        nfull = len(tiles)
        if self.half(l):
            nfull = 1 + (LLAT // 2) // 512 + 1
        seq = [(gi, ti) for gi in range(len(groups)) for ti in range(len(tiles)) if ti < nfull or groups[gi][3] in ('XBCT', 'DTR')]
        loadh(seq[0][1], 0)
        for si, (gi, ti) in enumerate(seq):
            kind, c0, ncols, dest = groups[gi]
            t0, n, wi = tiles[ti]
            if ti == 0 and gi + 1 < len(groups):
                loadw(gi + 1)
            assert ti != 0 or True
            if si + 1 < len(seq):
                loadh(seq[si + 1][1], si + 1)
            w = W[gi % 2]
            h = hT[si % 3]
            if kind in ('fm', 'sig'):
                nch = ncols // 128
                cnt['stg'] += 1
                sg_ = stg[cnt['stg'] % 2]
                for j in range(nch):
                    p_ = nps()
                    for kc in range(8):
                        P.op('pe', lambda e, p_=p_, j=j, kc=kc: e.matmul(p_.t[:, 0:n], w.t[:, kc, j * 128:(j + 1) * 128], h.t[:, kc, 0:n],
                                                                        start=(kc == 0), stop=(kc == 7)), [w.b, h.b], [p_.b])
                    if kind == 'sig':
                        P.op('act', lambda e, p_=p_, j=j: e.activation(out=sg_.t[:, j, 0:n], in_=p_.t[:, 0:n], func=AF.Sigmoid), [p_.b], [sg_.b])
                    elif j % 2 == 0:
                        P.op('dve', lambda e, p_=p_, j=j: e.tensor_copy(out=sg_.t[:, j, 0:n], in_=p_.t[:, 0:n]), [p_.b], [sg_.b])
                    else:
                        P.op('act', lambda e, p_=p_, j=j: e.activation(out=sg_.t[:, j, 0:n], in_=p_.t[:, 0:n], func=AF.Copy), [p_.b], [sg_.b])
                if kind == 'sig':
                    gidx = int(dest[2])
                    dd = kp(self.scr['GT'])[:, gidx * 8:(gidx + 1) * 8, t0:t0 + n]
                    dn = 'GT'
                else:
                    dd = kp(self.scr[dest])[:, :, t0:t0 + n]
                    dn = dest
                P.dma('pool', lambda e, dd=dd, nch=nch: e.dma_start(out=dd, in_=sg_.t[:, 0:nch, 0:n]), [sg_.b], [P.D(dn, (t0, gi))])
            elif kind == 'glu':
                cnt['stg'] += 1
                sg_ = stg[cnt['stg'] % 2]
                for j in range(4):
                    pa, pg = nps(), nps()
                    for (p_, jj) in ((pa, j), (pg, j + 4)):
                        for kc in range(8):
                            P.op('pe', lambda e, p_=p_, jj=jj, kc=kc: e.matmul(p_.t[:, 0:n], w.t[:, kc, jj * 128:(jj + 1) * 128], h.t[:, kc, 0:n],
                                                                              start=(kc == 0), stop=(kc == 7)), [w.b, h.b], [p_.b])
                    cnt['sg'] += 1
                    s2 = sg[cnt['sg'] % 2]
                    P.op('act', lambda e, pg=pg, s2=s2: e.activation(out=s2.t[:, 0:n], in_=pg.t[:, 0:n], func=AF.Sigmoid), [pg.b], [s2.b])
                    P.op('dve', lambda e, pa=pa, s2=s2, j=j: e.tensor_tensor(out=sg_.t[:, j, 0:n], in0=pa.t[:, 0:n], in1=s2.t[:, 0:n], op=ALU.mult),
                         [pa.b, s2.b], [sg_.b])
                dd = kp(self.scr['UT'])[:, :, t0:t0 + n]
                P.dma('pool', lambda e, dd=dd: e.dma_start(out=dd, in_=sg_.t[:, 0:4, 0:n]), [sg_.b], [P.D('UT', t0)])
            else:
                for sub in range(n // 128):
                    tt = t0 + sub * 128
                    cnt['stg'] += 1
                    if kind == 'z':
                        sg_ = stg[cnt['stg'] % 2]
                        for half in range(2):
                            p_ = nps()
                            for kc in range(8):
                                P.op('pe', lambda e, p_=p_, half=half, kc=kc, sub=sub: e.matmul(
                                    p_.t[:, 0:512], h.t[:, kc, sub * 128:(sub + 1) * 128], w.t[:, kc, half * 512:(half + 1) * 512],
                                    start=(kc == 0), stop=(kc == 7)), [w.b, h.b], [p_.b])
                            P.op('act', lambda e, p_=p_, half=half: e.activation(out=sg_.t[:, half, :], in_=p_.t[:, 0:512], func=AF.Silu), [p_.b], [sg_.b])
                        P.dma('pool', lambda e, tt=tt: e.dma_start(out=self.scr['SZ'][tt:tt + 128, :].rearrange("p (a b) -> p a b", a=2), in_=sg_.t[:, 0:2, :]), [sg_.b], [P.D('SZ', tt)])
                    elif kind == 'v':
                        sv_ = stgv[cnt['stg'] % 2]
                        p_ = nps()
                        for kc in range(8):
                            P.op('pe', lambda e, p_=p_, kc=kc, sub=sub: e.matmul(p_.t[:, 0:512], h.t[:, kc, sub * 128:(sub + 1) * 128], w.t[:, kc, 0:512],
                                                                              start=(kc == 0), stop=(kc == 7)), [w.b, h.b], [p_.b])
                        pv = p_.t[:, 0:512].rearrange("p (a b d) -> p a b d", a=4, b=2)
                        ov = sv_.t[:].rearrange("p (a b c d) -> p a b c d", a=4, b=2, c=2)
                        P.op('dve', lambda e, pv=pv, ov=ov: e.tensor_copy(out=ov[:, :, 0, 0, :], in_=pv[:, :, 0, :]), [p_.b], [sv_.b])
                        P.op('act', lambda e, pv=pv, ov=ov: e.activation(out=ov[:, :, 1, 1, :], in_=pv[:, :, 1, :], func=AF.Copy), [p_.b], [sv_.b])
                        P.dma('pool', lambda e, tt=tt, sv_=sv_: e.dma_start(out=self.scr['VP'][tt:tt + 128, :], in_=sv_.t[:]), [sv_.b], [P.D('VP', tt)])
                    else:
                        sd_ = stgd[cnt['stg'] % 2]
                        p_ = nps()
                        for kc in range(8):
                            P.op('pe', lambda e, p_=p_, kc=kc, sub=sub: e.matmul(p_.t[:, 0:32], h.t[:, kc, sub * 128:(sub + 1) * 128], w.t[:, kc, 0:32],
                                                                              start=(kc == 0), stop=(kc == 7)), [w.b, h.b], [p_.b])
                        P.op('dve', lambda e, p_=p_, sd_=sd_: e.tensor_copy(out=sd_.t[:], in_=p_.t[:, 0:32]), [p_.b], [sd_.b])
                        P.dma('pool', lambda e, tt=tt, sd_=sd_: e.dma_start(out=self.scr['DTR'][tt:tt + 128, :], in_=sd_.t[:]), [sd_.b], [P.D('DTR', tt)])

    def make_diag(self, st, nch, ntap, woff):
        P = self.P
        dg = self.sb(st, 'dg', [128, nch, ntap, 128], BF16)
        for c in range(nch):
            for j in range(ntap):
                eng = 'dve' if (c * ntap + j) % 2 == 0 else 'pool'
                P.op(eng, lambda e, c=c, j=j: e.tensor_scalar(out=dg.t[:, c, j, :], in0=self.idb.t[:], scalar1=self.pc.t[:, woff + c * ntap + j:woff + c * ntap + j + 1],
                                                             scalar2=None, op0=ALU.mult), [self.idb.b, self.pc.b], [dg.b])
        return dg

    def seg_bounds(self, t0):
        return (0, LCTX) if t0 < LCTX else (LCTX, T)

    def load_halo(self, xb, srcname, nch, t0, n, halo):
        P = self.P
        lo, hi = self.seg_bounds(t0)
        a, b = max(lo, t0 - halo), min(hi, t0 + n + halo)
        if a != t0 - halo or b != t0 + n + halo:
            P.op('pool', lambda e: e.memset(xb.t[:, :, 0:n + 2 * halo], 0.0), [], [xb.b])
        o = a - (t0 - halo)
        src = kp(self.scr[srcname])[:, :, a:b]
        P.dma('sp', lambda e: e.dma_start(out=xb.t[:, :, o:o + (b - a)], in_=src), [P.D(srcname, 0)], [xb.b])

    def ph_sconv(self, st, l, need_ctx):
        P = self.P
        tiles = token_tiles(True)
        dg = self.make_diag(st, 12, 5, O_SCW)
        xb = [self.sb(st, 'xb', [128, 12, 516], BF16) for _ in range(2)]
        ux = [self.sb(st, 'ux', [128, 12, 512], BF16) for _ in range(2)]
        xtok = [self.sb(st, 'xtok', [128, 1280], BF16) for _ in range(2)]
        pcv = [self.ps(st, 'pcv', [128, 512]) for _ in range(3)]
        ptx = [self.ps(st, 'ptx', [128, 1024], BF16) for _ in range(2)]
        ptb = [self.ps(st, 'ptb', [128, 256], BF16) for _ in range(2)]
        self.load_halo(xb[0], 'XBCT', 12, tiles[0][0], tiles[0][1], 2)
        k = 0
        for i, (t0, n, w) in enumerate(tiles):
            if i + 1 < len(tiles):
                self.load_halo(xb[(i + 1) % 2], 'XBCT', 12, tiles[i + 1][0], tiles[i + 1][1], 2)
            x_, u_ = xb[i % 2], ux[i % 2]
            for c in range(12):
                p_ = pcv[c % 3]
                for j in range(5):
                    P.op('pe', lambda e, p_=p_, c=c, j=j: e.matmul(p_.t[:, 0:n], dg.t[:, c, j, :], x_.t[:, c, j:j + n], start=(j == 0), stop=(j == 4)),
                         [dg.b, x_.b], [p_.b])
                P.op('act', lambda e, p_=p_, c=c: e.activation(out=u_.t[:, c, 0:n], in_=p_.t[:, 0:n], func=AF.Silu, bias=self.pc.t[:, O_SCB + c:O_SCB + c + 1]),
                     [p_.b, self.pc.b], [u_.b])
            P.dma('pool', lambda e, u_=u_, t0=t0, n=n: e.dma_start(out=kp(self.scr['BCT'])[:, :, t0:t0 + n], in_=u_.t[:, 8:12, 0:n]), [u_.b], [P.D('BCT', t0)])
            for sub in range(n // 128):
                k += 1
                px, pb, xt = ptx[k % 2], ptb[k % 2], xtok[k % 2]
                for c in range(10):
                    o_ = px.t[:, c * 128:(c + 1) * 128] if c < 8 else pb.t[:, (c - 8) * 128:(c - 7) * 128]
                    P.op('pe', lambda e, o_=o_, c=c, sub=sub: e.transpose(o_, u_.t[:, c, sub * 128:(sub + 1) * 128], self.idb.t[:]),
                         [u_.b, self.idb.b], [px.b if c < 8 else pb.b])
                P.op('dve', lambda e, px=px, xt=xt: e.tensor_copy(out=xt.t[:, 0:1024], in_=px.t[:]), [px.b], [xt.b])
                P.op('act', lambda e, pb=pb, xt=xt: e.activation(out=xt.t[:, 1024:1280], in_=pb.t[:, 0:256], func=AF.Copy), [pb.b], [xt.b])
                tt = t0 + sub * 128
                P.dma('pool', lambda e, xt=xt, tt=tt: e.dma_start(out=self.scr['XS'][tt:tt + 128, :], in_=xt.t[:, 0:1024]), [xt.b], [P.D('XS', tt)])
                P.dma('pool', lambda e, xt=xt, tt=tt: e.dma_start(out=self.scr['BMT'][tt:tt + 128, :], in_=xt.t[:, 1024:1280]), [xt.b], [P.D('BMT', tt)])

    def ph_ssd(self, st, l, need_ctx):
        P = self.P
        pc = self.pc
        A = self.sb(st, 'A', [128, 32], F32)
        P.op('act', lambda e: e.activation(out=A.t[:], in_=pc.t[:, O_ALOG:O_ALOG + 32], func=AF.Exp), [pc.b], [A.b])
        P.op('dve', lambda e: e.tensor_scalar(out=A.t[:], in0=A.t[:], scalar1=-1.0, scalar2=None, op0=ALU.mult), [A.b], [A.b])
        DI = self.sb(st, 'DI', [128, 16, 128], BF16)
        for e_ in range(16):
            P.op('dve', lambda e, e_=e_: e.tensor_scalar(out=DI.t[:, e_, :], in0=self.idb.t[:], scalar1=pc.t[:, O_SD + e_:O_SD + e_ + 1], scalar2=None, op0=ALU.mult),
                 [self.idb.b, pc.b], [DI.b])
        gn = self.sb(st, 'gn', [128, 8], F32)
        P.op('dve', lambda e: e.tensor_copy(out=gn.t[:], in_=pc.t[:, O_SNORM:O_SNORM + 8]), [pc.b], [gn.b])
        hst = self.sb(st, 'hst', [128, 1024], F32)
        hbf = self.sb(st, 'hbf', [128, 1024], BF16)
        nb = 3
        xs = [self.sb(st, 'xs', [128, 1024], BF16) for _ in range(nb)]
        bmt = [self.sb(st, 'bmt', [128, 256], BF16) for _ in range(nb)]
        bct = [self.sb(st, 'bct', [128, 4, 128], BF16) for _ in range(nb)]
        dtr = [self.sb(st, 'dtr', [128, 32], F32) for _ in range(nb)]
        yfb = [self.sb(st, 'yfb', [128, 1024], F32) for _ in range(nb)]
        szb = [self.sb(st, 'szb', [128, 1024], BF16) for _ in range(nb)]
        sms = [{k: self.sb(st, k, [128, 16], F32) for k in ('t1', 'dt', 'dta', 'ncs', 'ecs', 'cd', 'w', 'wd')} for _ in range(2)]
        cs_sbs = [self.sb(st, 'cs_sb', [128, 32], F32) for _ in range(2)]
        csTs = [self.sb(st, 'csT', [16, 128], F32) for _ in range(2)]
        scTs = [self.sb(st, 'scT', [128, 2, 128], F32) for _ in range(2)]
        eL = [self.sb(st, 'eL', [128, 128], F32) for _ in range(4)]
        Malls = [self.sb(st, 'Mall', [128, 16, 128], BF16) for _ in range(2)]
        negb = self.sb(st, 'negb', [128, 2, 128], BF16)
        P.op('dve', lambda e: e.tensor_copy(out=negb.t[:, 0, :], in_=self.C(C_NEGF)), [self.cst.b], [negb.b])
        P.op('dve', lambda e: e.tensor_copy(out=negb.t[:, 1, :], in_=self.C(C_NEGB)), [self.cst.b], [negb.b])
        xw = self.sb(st, 'xw', [128, 1024], BF16)
        ytmp = self.sb(st, 'ytmp', [128, 1024], F32)
        ysq = self.sb(st, 'ysq', [128, 1024], F32)
        ynb = self.sb(st, 'ynb', [128, 1024], BF16)
        ynT = [self.sb(st, 'ynT', [128, 8, 128], BF16) for _ in range(2)]
        ssq = self.sb(st, 'ssq', [128, 1], F32)
        psA = self.ps(st, 'psA', [128, 512])
        psLs = [self.ps(st, 'psL', [128, 512]) for _ in range(2)]
        psYd = [self.ps(st, 'psYd', [128, 512]) for _ in range(2)]
        psYo = [self.ps(st, 'psYo', [128, 512]) for _ in range(2)]
        psT = self.ps(st, 'psT', [128, 1024], BF16)
        idn32, ones32 = self.C(C_IDN), self.C(C_ONES)
        cb = self.cst.b

        hf = self.half(l)
        nown = (LLAT // 2) // 128

        def chunk_list(direction):
            ctxc = [0, 128]
            lat = [LCTX + 128 * i for i in range(LLAT // 128)]
            if hf and direction == 0:
                lat = lat[:nown]
            return ctxc + lat if direction == 0 else ctxc[::-1] + lat[::-1]

        for d in range(2):
            tri = self.C(C_TRIF if d == 0 else C_TRIB)
            P.op('dve', lambda e: e.memset(hst.t[:], 0.0), [], [hst.b])
            P.op('act', lambda e: e.activation(out=hbf.t[:], in_=hst.t[:], func=AF.Copy), [hst.b], [hbf.b])
            chunks = chunk_list(d)

            def wanty(tc0):
                return ((tc0 >= LCTX) or need_ctx) and not (hf and tc0 >= LCTX + nown * 128)

            def load(ci):
                tc0 = chunks[ci]
                s_ = ci % nb
                P.dma('sp', lambda e: e.dma_start(out=xs[s_].t[:], in_=self.scr['XS'][tc0:tc0 + 128, :]), [P.D('XS', tc0)], [xs[s_].b])
                P.dma('sp', lambda e: e.dma_start(out=bmt[s_].t[:], in_=self.scr['BMT'][tc0:tc0 + 128, :]), [P.D('BMT', tc0)], [bmt[s_].b])
                P.dma('sp', lambda e: e.dma_start(out=bct[s_].t[:], in_=kp(self.scr['BCT'])[:, :, tc0:tc0 + 128]), [P.D('BCT', 0)], [bct[s_].b])
                P.dma('sp', lambda e: e.dma_start(out=dtr[s_].t[:], in_=self.scr['DTR'][tc0:tc0 + 128, :]), [P.D('DTR', tc0)], [dtr[s_].b])
                if d == 1 and wanty(tc0):
                    P.dma('sp', lambda e: e.dma_start(out=yfb[s_].t[:], in_=self.scr['YF'][tc0:tc0 + 128, :]), [P.D('YF', tc0)], [yfb[s_].b])
                    P.dma('sp', lambda e: e.dma_start(out=szb[s_].t[:], in_=self.scr['SZ'][tc0:tc0 + 128, :]), [P.D('SZ', tc0)], [szb[s_].b])

            def stageA(ci):
                tc0 = chunks[ci]
                s_ = ci % nb
                BC, DT = bct[s_], dtr[s_]
                sm, cs_sb, csT, scT, Mall = sms[ci % 2], cs_sbs[ci % 2], csTs[ci % 2], scTs[ci % 2], Malls[ci % 2]
                t1, dt, dta, ncs, ecs, cd, w_, wd = (sm[k] for k in ('t1', 'dt', 'dta', 'ncs', 'ecs', 'cd', 'w', 'wd'))
                P.op('dve', lambda e: e.tensor_tensor(out=t1.t[:], in0=DT.t[:, d * 16:(d + 1) * 16], in1=pc.t[:, O_DTB + d * 16:O_DTB + (d + 1) * 16], op=ALU.add),
                     [DT.b, pc.b], [t1.b])
                P.op('act', lambda e: e.activation(out=t1.t[:], in_=t1.t[:], func=AF.Exp), [t1.b], [t1.b])
                P.op('act', lambda e: e.activation(out=dt.t[:], in_=t1.t[:], func=AF.Ln, bias=1.0), [t1.b], [dt.b])
                P.op('dve', lambda e: e.tensor_tensor(out=dta.t[:], in0=dt.t[:], in1=A.t[:, d * 16:(d + 1) * 16], op=ALU.mult), [dt.b, A.b], [dta.b])
                P.op('pe', lambda e: e.matmul(psA.t[:, 0:16], tri, dta.t[:], start=True, stop=True), [cb, dta.b], [psA.b])
                P.op('pe', lambda e: e.matmul(psA.t[:, 16:32], ones32, dta.t[:], start=True, stop=True), [cb, dta.b], [psA.b])
                wy = wanty(tc0)
                if wy:
                    P.op('pe', lambda e: e.matmul(psA.t[0:16, 32:160], dta.t[:], tri, start=True, stop=True), [cb, dta.b], [psA.b])
                    for g in range(2):
                        P.op('pe', lambda e, g=g: e.matmul(psA.t[:, 160 + g * 128:160 + (g + 1) * 128], BC.t[:, g, :], BC.t[:, 2 + g, :], start=True, stop=True),
                             [BC.b], [psA.b])
                P.op('dve', lambda e: e.tensor_copy(out=cs_sb.t[:], in_=psA.t[:, 0:32]), [psA.b], [cs_sb.b])
                if wy:
                    P.op('act', lambda e: e.activation(out=csT.t[:], in_=psA.t[0:16, 32:160], func=AF.Copy), [psA.b], [csT.b])
                    P.op('act', lambda e: e.activation(out=scT.t[:].rearrange("p a b -> p (a b)"), in_=psA.t[:, 160:416], func=AF.Copy), [psA.b], [scT.b])
                P.op('dve', lambda e: e.tensor_scalar(out=ncs.t[:], in0=cs_sb.t[:, 0:16], scalar1=-1.0, scalar2=None, op0=ALU.mult), [cs_sb.b], [ncs.b])
                P.op('act', lambda e: e.activation(out=ecs.t[:], in_=cs_sb.t[:, 0:16], func=AF.Exp), [cs_sb.b], [ecs.b])
                P.op('act', lambda e: e.activation(out=cd.t[:], in_=cs_sb.t[:, 16:32], func=AF.Exp), [cs_sb.b], [cd.b])
                P.op('dve', lambda e: e.tensor_tensor(out=wd.t[:], in0=cs_sb.t[:, 16:32], in1=cs_sb.t[:, 0:16], op=ALU.subtract), [cs_sb.b], [wd.b])
                P.op('act', lambda e: e.activation(out=wd.t[:], in_=wd.t[:], func=AF.Exp), [wd.b], [wd.b])
                P.op('dve', lambda e: e.tensor_tensor(out=w_.t[:], in0=wd.t[:], in1=dt.t[:], op=ALU.mult), [wd.b, dt.b], [w_.b])
                if not wy:
                    return
                for e_ in range(16):
                    g = e_ // 8
                    sl = e_ % 4
                    psL = psLs[e_ % 2]
                    P.op('pe', lambda e, e_=e_: e.matmul(psL.t[:, 0:128], self.selc.t[0:16, e_ * 128:(e_ + 1) * 128], csT.t[:], start=True, stop=False),
                         [self.selc.b, csT.b], [psL.b])
                    P.op('pe', lambda e: e.matmul(psL.t[:, 0:128], self.idb.t[:], negb.t[:, d, :], start=False, stop=True), [self.idb.b, negb.b], [psL.b])
                    P.op('act', lambda e, e_=e_, sl=sl: e.activation(out=eL[sl].t[:], in_=psL.t[:, 0:128], func=AF.Exp, bias=ncs.t[:, e_:e_ + 1]),
                         [psL.b, ncs.b], [eL[sl].b])
                    P.op('dve', lambda e, e_=e_, sl=sl, g=g: e.scalar_tensor_tensor(out=Mall.t[:, e_, :], in0=eL[sl].t[:], scalar=dt.t[:, e_:e_ + 1], in1=scT.t[:, g, :],
                                                                               op0=ALU.mult, op1=ALU.mult), [eL[sl].b, dt.b, scT.b], [Mall.b])

            def stageB(ci):
                tc0 = chunks[ci]
                s_ = ci % nb
                X, BM, BC = xs[s_], bmt[s_], bct[s_]
                sm, Mall = sms[ci % 2], Malls[ci % 2]
                ecs, cd, w_ = sm['ecs'], sm['cd'], sm['w']
                if wanty(tc0):
                    for e_ in range(16):
                        g = e_ // 8
                        o_ = psYd[g].t[:, (e_ % 8) * 64:(e_ % 8 + 1) * 64]
                        P.op('pe', lambda e, o_=o_, e_=e_: e.matmul(o_, Mall.t[:, e_, :], X.t[:, e_ * 64:(e_ + 1) * 64], start=True, stop=(d == 1)),
                             [Mall.b, X.b], [psYd[g].b])
                        if d == 0:
                            P.op('pe', lambda e, o_=o_, e_=e_: e.matmul(o_, DI.t[:, e_, :], X.t[:, e_ * 64:(e_ + 1) * 64], start=False, stop=True),
                                 [DI.b, X.b], [psYd[g].b])
                    for g in range(2):
                        P.op('pe', lambda e, g=g: e.matmul(psYo[g].t[:], BC.t[:, 2 + g, :], hbf.t[:, g * 512:(g + 1) * 512], start=True, stop=True),
                             [BC.b, hbf.b], [psYo[g].b])
                    for g in range(2):
                        yv = ytmp.t[:, g * 512:(g + 1) * 512]
                        P.op('dve', lambda e, g=g, yv=yv: e.tensor_tensor(out=yv.rearrange("p (a b) -> p a b", b=64), in0=psYo[g].t[:].rearrange("p (a b) -> p a b", b=64),
                                                                         in1=ecs.t[:, g * 8:(g + 1) * 8].unsqueeze(2).to_broadcast([128, 8, 64]), op=ALU.mult),
                             [psYo[g].b, ecs.b], [ytmp.b])
                        P.op('dve', lambda e, g=g, yv=yv: e.tensor_tensor(out=yv, in0=yv, in1=psYd[g].t[:], op=ALU.add), [psYd[g].b, ytmp.b], [ytmp.b])
                    if d == 0:
                        P.dma('pool', lambda e: e.dma_start(out=self.scr['YF'][tc0:tc0 + 128, :], in_=ytmp.t[:]), [ytmp.b], [P.D('YF', tc0)])
                    else:
                        YFb, SZb = yfb[s_], szb[s_]
                        P.op('pool', lambda e: e.tensor_tensor(out=ytmp.t[:], in0=ytmp.t[:], in1=YFb.t[:], op=ALU.add), [ytmp.b, YFb.b], [ytmp.b])
                        P.op('pool', lambda e: e.tensor_tensor(out=ytmp.t[:], in0=ytmp.t[:], in1=SZb.t[:], op=ALU.mult), [ytmp.b, SZb.b], [ytmp.b])
                        P.op('pool', lambda e: e.tensor_tensor(out=ysq.t[:], in0=ytmp.t[:], in1=ytmp.t[:], op=ALU.mult), [ytmp.b], [ysq.b])
                        P.op('dve', lambda e: e.reduce_sum(out=ssq.t[:], in_=ysq.t[:], axis=AX.X), [ysq.b], [ssq.b])
                        P.op('act', lambda e: e.activation(out=ssq.t[:], in_=ssq.t[:], func=AF.Sqrt, scale=1.0 / D, bias=EPS), [ssq.b], [ssq.b])
                        P.op('dve', lambda e: e.reciprocal(out=ssq.t[:], in_=ssq.t[:]), [ssq.b], [ssq.b])
                        P.op('act', lambda e: e.activation(out=ynb.t[:], in_=ytmp.t[:], func=AF.Identity, scale=ssq.t[:, 0:1]), [ytmp.b, ssq.b], [ynb.b])
                        for c in range(8):
                            P.op('pe', lambda e, c=c: e.transpose(psT.t[:, c * 128:(c + 1) * 128], ynb.t[:, c * 128:(c + 1) * 128], self.idb.t[:]),
                                 [ynb.b, self.idb.b], [psT.b])
                        yt_ = ynT[ci % 2]
                        P.op('dve', lambda e: e.tensor_tensor(out=yt_.t[:], in0=psT.t[:].rearrange("p (a b) -> p a b", b=128),
                                                             in1=gn.t[:].unsqueeze(2).to_broadcast([128, 8, 128]), op=ALU.mult), [psT.b, gn.b], [yt_.b])
                        P.dma('pool', lambda e: e.dma_start(out=kp(self.scr['YNT'])[:, :, tc0:tc0 + 128], in_=yt_.t[:]), [yt_.b], [P.D('YNT', tc0)])
                P.op('pool', lambda e: e.tensor_tensor(out=xw.t[:].rearrange("p (a b) -> p a b", b=64), in0=X.t[:].rearrange("p (a b) -> p a b", b=64),
                                                      in1=w_.t[:].unsqueeze(2).to_broadcast([128, 16, 64]), op=ALU.mult), [X.b, w_.b], [xw.b])
                for g in range(2):
                    P.op('pe', lambda e, g=g: e.matmul(psYo[g].t[:], BM.t[:, g * 128:(g + 1) * 128], xw.t[:, g * 512:(g + 1) * 512], start=True, stop=True),
                         [BM.b, xw.b], [psYo[g].b])
                P.op('pool', lambda e: e.tensor_tensor(out=hst.t[:].rearrange("p (a b) -> p a b", b=64), in0=hst.t[:].rearrange("p (a b) -> p a b", b=64),
                                                      in1=cd.t[:].unsqueeze(2).to_broadcast([128, 16, 64]), op=ALU.mult), [hst.b, cd.b], [hst.b])
                for g in range(2):
                    P.op('dve', lambda e, g=g: e.tensor_tensor(out=hst.t[:, g * 512:(g + 1) * 512], in0=hst.t[:, g * 512:(g + 1) * 512], in1=psYo[g].t[:], op=ALU.add),
                         [hst.b, psYo[g].b], [hst.b])
                P.op('act', lambda e: e.activation(out=hbf.t[:], in_=hst.t[:], func=AF.Copy), [hst.b], [hbf.b])

            n_ = len(chunks)
            load(0)
            if n_ > 1:
                load(1)
            stageA(0)
            for ci in range(n_):
                if ci + 2 < n_:
                    load(ci + 2)
                if ci + 1 < n_:
                    stageA(ci + 1)
                stageB(ci)

    def ph_na(self, st, l, need_ctx):
        P = self.P
        SC = 0.125
        kc_ = self.sb(st, 'kc', [128, 4, 256], BF16)
        vc_ = self.sb(st, 'vc', [128, 2, 1024], BF16)
        P.dma('sp', lambda e: e.dma_start(out=kc_.t[:], in_=kp(self.scr['KT'])[:, :, 0:LCTX]), [P.D('KT', 0)], [kc_.b])
        P.dma('sp', lambda e: e.dma_start(out=vc_.t[:], in_=self.scr['VP'][0:LCTX, :].rearrange("(j p) n -> p j n", p=128)), [P.D('VP', 0)], [vc_.b])
        oa = [self.sb(st, 'oa', [128, 128], BF16) for _ in range(2)]
        for par in range(2):
            P.op('dve', lambda e, par=par: e.memset(oa[par].t[:], 0.0), [], [oa[par].b])
            P.op('dve', lambda e, par=par: e.memset(oa[par].t[:, par * 64:(par + 1) * 64], 1.0), [oa[par].b], [oa[par].b])
        bias = self.sb(st, 'bias', [128, 8, 448], F32)
        P.op('dve', lambda e: e.memset(bias.t[:], 0.0), [], [bias.b])
        kw = [self.sb(st, 'kw', [128, 4, 640], BF16) for _ in range(2)]
        vw = [self.sb(st, 'vw', [128, 5, 1024], BF16) for _ in range(2)]
        qr = [self.sb(st, 'qr', [128, 4, 64], BF16) for _ in range(2)]
        e1 = [self.sb(st, 'e1', [128, 448], F32) for _ in range(2)]
        pw = [self.sb(st, 'pw', [128, 448], BF16) for _ in range(3)]
        rec = [self.sb(st, 'rec', [128, 64], F32) for _ in range(2)]
        nao = [self.sb(st, 'nao', [128, 4, 512], BF16) for _ in range(2)]
        psS = [self.ps(st, 'psS', [128, 512]) for _ in range(3)]
        psO = [self.ps(st, 'psO', [128, 64]) for _ in range(2)]
        psM = [self.ps(st, 'psM', [128, 64]) for _ in range(2)]
        rows = []
        if need_ctx:
            rows += [('ctx', i) for i in range(4)]
        rows += [('lat', r) for r in range(64 if self.half(l) else 128)]
        cur_var = [None]
        cnt = {'s': 0, 'p': 0, 'o': 0, 'e': 0}

        def load(ri):
            kind, r = rows[ri]
            s_ = ri % 2
            tq0 = r * 64 if kind == 'ctx' else LCTX + r * 64
            P.dma('sp', lambda e: e.dma_start(out=qr[s_].t[:], in_=kp(self.scr['QT'])[:, :, tq0:tq0 + 64]), [P.D('QT', 0)], [qr[s_].b])
            if kind == 'lat':
                rs = min(max(r - 4, 0), 118)
                k0 = LCTX + rs * 64
                P.dma('sp', lambda e: e.dma_start(out=kw[s_].t[:], in_=kp(self.scr['KT'])[:, :, k0:k0 + 640]), [P.D('KT', 0)], [kw[s_].b])
                P.dma('sp', lambda e: e.dma_start(out=vw[s_].t[:], in_=self.scr['VP'][k0:k0 + 640, :].rearrange("(j p) n -> p j n", p=128)), [P.D('VP', 0)], [vw[s_].b])

        units = []
        for ri, (kind, r) in enumerate(rows):
            for hp in range(4):
                for par in range(2):
                    units.append((ri, kind, r, hp, par))
        state = {}

        def stage1(ui):
            ri, kind, r, hp, par = units[ui]
            s_ = ri % 2
            Q, KW = qr[s_], kw[s_]
            if hp == 0 and par == 0:
                if kind == 'lat':
                    var = r if r < 5 else (5 if r <= 121 else 6 + (r - 122))
                    if cur_var[0] != var:
                        cur_var[0] = var
                        P.dma('sp', lambda e, var=var: e.dma_start(out=bias.t[:, :, 0:320], in_=self.inp['nabias'][l, var].rearrange("p (a b) -> p a b", a=8)), [], [bias.b])
            nwin = 5 if kind == 'lat' else 0
            h = hp * 2 + par
            pr = slice(par * 64, (par + 1) * 64)
            pS = psS[ui % 3]
            PW = pw[ui % 3]
            for j in range(nwin):
                P.op('pe', lambda e, j=j: e.matmul(pS.t[:, j * 64:(j + 1) * 64], KW.t[pr, hp, j * 128:(j + 1) * 128], Q.t[pr, hp, :], start=True, stop=True), [KW.b, Q.b], [pS.b])
            for j in range(2):
                P.op('pe', lambda e, j=j: e.matmul(pS.t[:, 320 + j * 64:320 + (j + 1) * 64], kc_.t[pr, hp, j * 128:(j + 1) * 128], Q.t[pr, hp, :], start=True, stop=True), [kc_.b, Q.b], [pS.b])
            if nwin:
                E1 = e1[ui % 2]
                P.op('dve', lambda e: e.scalar_tensor_tensor(out=E1.t[:], in0=pS.t[:, 0:448], scalar=SC, in1=bias.t[:, h, :], op0=ALU.mult, op1=ALU.add), [pS.b, bias.b], [E1.b])
                P.op('act', lambda e: e.activation(out=PW.t[:, 0:448], in_=E1.t[:], func=AF.Exp), [E1.b], [PW.b])
            else:
                P.op('act', lambda e: e.activation(out=PW.t[:, 320:448], in_=pS.t[:, 320:448], func=AF.Exp, scale=SC), [pS.b], [PW.b])

        def stage2(ui):
            ri, kind, r, hp, par = units[ui]
            s_ = ri % 2
            VW = vw[s_]
            nwin = 5 if kind == 'lat' else 0
            h = hp * 2 + par
            PW = pw[ui % 3]
            oi = (ui // 2) % 2
            pO, pM = psO[oi], psM[oi]
            nmm = 2 * (nwin + 2)
            for j in range(nwin + 2):
                if j < nwin:
                    vap, vb, pap = VW.t[:, j, h * 128:(h + 1) * 128], VW.b, PW.t[:, j * 64:(j + 1) * 64]
                else:
                    vap, vb = vc_.t[:, j - nwin, h * 128:(h + 1) * 128], vc_.b
                    pap = PW.t[:, 320 + (j - nwin) * 64:320 + (j - nwin + 1) * 64]
                mi = par * (nwin + 2) + j
                first, last = (mi == 0), (mi == nmm - 1)
                P.op('pe', lambda e, vap=vap, pap=pap, first=first, last=last: e.matmul(pO.t[:, 0:64], vap, pap, start=first, stop=last), [vb, PW.b], [pO.b])
                P.op('pe', lambda e, pap=pap, first=first, last=last: e.matmul(pM.t[:, 0:64], oa[par].t[:], pap, start=first, stop=last), [oa[par].b, PW.b], [pM.b])
            if par == 1:
                if kind == 'ctx':
                    blk, NO, tb, nn = r, nao[0], 0, 256
                else:
                    blk, NO, tb, nn = r % 8, nao[(1 + r // 8) % 2], LCTX + (r // 8) * 512, 512
                R_ = rec[oi]
                P.op('dve', lambda e: e.reciprocal(out=R_.t[:], in_=pM.t[:, 0:64]), [pM.b], [R_.b])
                P.op('dve', lambda e: e.tensor_tensor(out=NO.t[:, hp, blk * 64:(blk + 1) * 64], in0=pO.t[:, 0:64], in1=R_.t[:], op=ALU.mult), [pO.b, R_.b], [NO.b])
                last_of_tile = hp == 3 and ((kind == 'ctx' and r == 3) or (kind == 'lat' and blk == 7))
                if last_of_tile:
                    P.dma('pool', lambda e: e.dma_start(out=kp(self.scr['NAT'])[:, :, tb:tb + nn], in_=NO.t[:, :, 0:nn]), [NO.b], [P.D('NAT', tb)])

        load(0)
        for ui in range(len(units) + 1):
            if ui < len(units):
                stage1(ui)
            if ui >= 1:
                stage2(ui - 1)
            if ui < len(units) and units[ui][3] == 0 and units[ui][4] == 0 and units[ui][0] + 1 < len(rows):
                load(units[ui][0] + 1)

    def ph_conf(self, st, l, need_ctx):
        P = self.P
        pc = self.pc
        tiles = self.own_tiles(l, need_ctx)
        dg = self.make_diag(st, 4, 31, O_CCW)
        ub = [self.sb(st, 'ub', [128, 4, 542], BF16) for _ in range(2)]
        u32 = self.sb(st, 'u32', [128, 4, 512], F32)
        usq = self.sb(st, 'usq', [128, 4, 512], F32)
        mean = self.sb(st, 'mean', [128, 512], F32)
        rstd = self.sb(st, 'rstd', [128, 512], F32)
        tmp = self.sb(st, 'tmp', [128, 512], F32)
        cf = [self.sb(st, 'cf', [128, 4, 512], BF16) for _ in range(2)]
        pcv = [self.ps(st, 'pcv', [128, 512]) for _ in range(2)]
        psm = self.ps(st, 'psm', [128, 512])
        psq = self.ps(st, 'psq', [128, 512])
        ones32 = self.C(C_ONES)
        self.load_halo(ub[0], 'UT', 4, tiles[0][0], tiles[0][1], 15)
        for i, (t0, n, w) in enumerate(tiles):
            if i + 1 < len(tiles):
                self.load_halo(ub[(i + 1) % 2], 'UT', 4, tiles[i + 1][0], tiles[i + 1][1], 15)
            U = ub[i % 2]
            for c in range(4):
                p_ = pcv[c % 2]
                for j in range(31):
                    P.op('pe', lambda e, p_=p_, c=c, j=j: e.matmul(p_.t[:, 0:n], dg.t[:, c, j, :], U.t[:, c, j:j + n], start=(j == 0), stop=(j == 30)),
                         [dg.b, U.b], [p_.b])
                P.op('act', lambda e, p_=p_, c=c: e.activation(out=u32.t[:, c, 0:n], in_=p_.t[:, 0:n], func=AF.Identity, bias=pc.t[:, O_CCB + c:O_CCB + c + 1]),
                     [p_.b, pc.b], [u32.b])
            P.op('pool', lambda e: e.tensor_tensor(out=usq.t[:, :, 0:n], in0=u32.t[:, :, 0:n], in1=u32.t[:, :, 0:n], op=ALU.mult), [u32.b], [usq.b])
            for c in range(4):
                P.op('pe', lambda e, c=c: e.matmul(psm.t[:, 0:n], ones32, u32.t[:, c, 0:n], start=(c == 0), stop=(c == 3)), [self.cst.b, u32.b], [psm.b])
            for c in range(4):
                P.op('pe', lambda e, c=c: e.matmul(psq.t[:, 0:n], ones32, usq.t[:, c, 0:n], start=(c == 0), stop=(c == 3)), [self.cst.b, usq.b], [psq.b])
            P.op('dve', lambda e: e.tensor_scalar(out=mean.t[:, 0:n], in0=psm.t[:, 0:n], scalar1=1.0 / 512, scalar2=None, op0=ALU.mult), [psm.b], [mean.b])
            P.op('dve', lambda e: e.tensor_tensor(out=tmp.t[:, 0:n], in0=mean.t[:, 0:n], in1=mean.t[:, 0:n], op=ALU.mult), [mean.b], [tmp.b])
            P.op('dve', lambda e: e.scalar_tensor_tensor(out=rstd.t[:, 0:n], in0=psq.t[:, 0:n], scalar=1.0 / 512, in1=tmp.t[:, 0:n], op0=ALU.mult, op1=ALU.subtract),
                 [psq.b, tmp.b], [rstd.b])
            P.op('act', lambda e: e.activation(out=rstd.t[:, 0:n], in_=rstd.t[:, 0:n], func=AF.Sqrt, bias=EPS), [rstd.b], [rstd.b])
            P.op('dve', lambda e: e.reciprocal(out=rstd.t[:, 0:n], in_=rstd.t[:, 0:n]), [rstd.b], [rstd.b])
            CF = cf[i % 2]
            for c in range(4):
                eng = 'dve' if c % 2 == 0 else 'pool'
                P.op(eng, lambda e, c=c: e.tensor_tensor(out=u32.t[:, c, 0:n], in0=u32.t[:, c, 0:n], in1=mean.t[:, 0:n], op=ALU.subtract), [u32.b, mean.b], [u32.b])
                P.op(eng, lambda e, c=c: e.tensor_tensor(out=u32.t[:, c, 0:n], in0=u32.t[:, c, 0:n], in1=rstd.t[:, 0:n], op=ALU.mult), [u32.b, rstd.b], [u32.b])
                P.op('act', lambda e, c=c: e.activation(out=CF.t[:, c, 0:n], in_=u32.t[:, c, 0:n], func=AF.Silu, scale=pc.t[:, O_LNG + c:O_LNG + c + 1],
                                                        bias=pc.t[:, O_LNB + c:O_LNB + c + 1]), [u32.b, pc.b], [CF.b])
            P.dma('pool', lambda e, CF=CF, t0=t0, n=n: e.dma_start(out=kp(self.scr['CFT'])[:, :, t0:t0 + n], in_=CF.t[:, :, 0:n]), [CF.b], [P.D('CFT', t0)])

    def ph_merge(self, st, l, need_ctx):
        P = self.P
        moe = (l % 2 == 1)
        tiles = self.own_tiles(l, need_ctx, 256)
        Wso = self.sb(st, 'Wso', [128, 8, 1024], BF16)
        Wno = self.sb(st, 'Wno', [128, 4, 1024], BF16)
        Wco = self.sb(st, 'Wco', [128, 4, 1024], BF16)
        Wo = self.sb(st, 'Wo', [128, 8, 1024], BF16)
        for (wt, nm) in ((Wso, 'ssd_out'), (Wno, 'na_out'), (Wco, 'conf_out'), (Wo, 'w_o')):
            P.dma('pool', lambda e, wt=wt, nm=nm: e.dma_start(out=wt.t[:], in_=kp(self.inp[nm][l])), [], [wt.b])
        yn = [self.sb(st, 'yn', [128, 8, 256], BF16) for _ in range(2)]
        na = [self.sb(st, 'na', [128, 4, 256], BF16) for _ in range(2)]
        cf = [self.sb(st, 'cf', [128, 4, 256], BF16) for _ in range(2)]
        gt = [self.sb(st, 'gt', [128, 24, 256], BF16) for _ in range(2)]
        xT = [self.sb(st, 'xT', [128, 8, 256], F32) for _ in range(2)]
        mg = self.sb(st, 'mg', [128, 8, 256], BF16)
        m1s = [self.sb(st, 'm1', [128, 256], F32) for _ in range(2)]
        m2s = [self.sb(st, 'm2', [128, 256], F32) for _ in range(2)]
        sq = self.sb(st, 'sq', [128, 8, 256], BF16)
        rst = self.sb(st, 'rst', [128, 256], F32)
        tmp = self.sb(st, 'tmp', [128, 256], F32)
        h2 = [self.sb(st, 'h2', [128, 8, 256], BF16) for _ in range(2)]
        pA = [self.ps(st, 'pA', [128, 256]) for _ in range(2)]
        pBs = [self.ps(st, 'pB', [128, 256]) for _ in range(2)]
        pCs = [self.ps(st, 'pC', [128, 256]) for _ in range(2)]
        pW = [self.ps(st, 'pW', [128, 256]) for _ in range(1)]
        pN = pW[0]
        if moe:
            h2f = self.sb(st, 'h2f', [128, 8, 256], F32)
            wr = self.sb(st, 'wr', [128, 8, 8], F32)
            P.dma('sp', lambda e: e.dma_start(out=wr.t[:], in_=kp(self.inp['moe_router'][0])), [], [wr.b])
            lg = self.sb(st, 'lg', [128, 8], F32)
            r8 = {k: self.sb(st, k, [128, 8], F32) for k in ('eq', 'l2', 'sel', 'ex', 'cmb')}
            r1 = {k: self.sb(st, k, [128, 1], F32) for k in ('mx1', 'nm1', 'mx2', 'ss')}
            cT = [self.sb(st, 'cT', [8, 128], F32) for _ in range(2)]
            pR = self.ps(st, 'pR', [128, 8])
            pRT = pR
        else:
            h2f = None

        def load(i):
            t0, n, w = tiles[i]
            s_ = i % 2
            P.dma('sp', lambda e: e.dma_start(out=yn[s_].t[:, :, 0:n], in_=kp(self.scr['YNT'])[:, :, t0:t0 + n]), [P.D('YNT', 0)], [yn[s_].b])
            P.dma('sp', lambda e: e.dma_start(out=na[s_].t[:, :, 0:n], in_=kp(self.scr['NAT'])[:, :, t0:t0 + n]), [P.D('NAT', 0)], [na[s_].b])
            P.dma('sp', lambda e: e.dma_start(out=cf[s_].t[:, :, 0:n], in_=kp(self.scr['CFT'])[:, :, t0:t0 + n]), [P.D('CFT', 0)], [cf[s_].b])
            P.dma('sp', lambda e: e.dma_start(out=gt[s_].t[:, :, 0:n], in_=kp(self.scr['GT'])[:, :, t0:t0 + n]), [P.D('GT', 0)], [gt[s_].b])
            P.dma('sp', lambda e: e.dma_start(out=xT[s_].t[:, :, 0:n], in_=kp(self.xsrc)[:, :, t0:t0 + n]), [P.D(self.xsrc_name, t0)], [xT[s_].b])

        load(0)
        for i, (t0, n, w) in enumerate(tiles):
            if i + 1 < len(tiles):
                load(i + 1)
            s_ = i % 2
            YN, NA, CF, GT, XT = yn[s_], na[s_], cf[s_], gt[s_], xT[s_]
            for fo in range(8):
                a_ = pA[fo % 2]
                pB, pC = pBs[fo % 2], pCs[fo % 2]
                m1, m2 = m1s[fo % 2], m2s[fo % 2]
                fs = slice(fo * 128, (fo + 1) * 128)
                for kc in range(8):
                    P.op('pe', lambda e, a_=a_, kc=kc, fs=fs: e.matmul(a_.t[:, 0:n], Wso.t[:, kc, fs], YN.t[:, kc, 0:n], start=(kc == 0), stop=(kc == 7)), [Wso.b, YN.b], [a_.b])
                for kc in range(4):
                    P.op('pe', lambda e, kc=kc, fs=fs: e.matmul(pB.t[:, 0:n], Wno.t[:, kc, fs], NA.t[:, kc, 0:n], start=(kc == 0), stop=(kc == 3)), [Wno.b, NA.b], [pB.b])
                for kc in range(4):
                    P.op('pe', lambda e, kc=kc, fs=fs: e.matmul(pC.t[:, 0:n], Wco.t[:, kc, fs], CF.t[:, kc, 0:n], start=(kc == 0), stop=(kc == 3)), [Wco.b, CF.b], [pC.b])
                P.op('dve', lambda e, a_=a_, fo=fo: e.tensor_tensor(out=m1.t[:, 0:n], in0=a_.t[:, 0:n], in1=GT.t[:, fo, 0:n], op=ALU.mult), [a_.b, GT.b], [m1.b])
                P.op('dve', lambda e, fo=fo: e.tensor_tensor(out=m2.t[:, 0:n], in0=pB.t[:, 0:n], in1=GT.t[:, 8 + fo, 0:n], op=ALU.mult), [pB.b, GT.b], [m2.b])
                P.op('pool', lambda e: e.tensor_tensor(out=m1.t[:, 0:n], in0=m1.t[:, 0:n], in1=m2.t[:, 0:n], op=ALU.add), [m1.b, m2.b], [m1.b])
                P.op('dve', lambda e, fo=fo: e.tensor_tensor(out=m2.t[:, 0:n], in0=pC.t[:, 0:n], in1=GT.t[:, 16 + fo, 0:n], op=ALU.mult), [pC.b, GT.b], [m2.b])
                P.op('pool', lambda e, fo=fo: e.tensor_tensor(out=mg.t[:, fo, 0:n], in0=m1.t[:, 0:n], in1=m2.t[:, 0:n], op=ALU.add), [m1.b, m2.b], [mg.b])
            for fo in range(8):
                p_ = pW[0]
                fs = slice(fo * 128, (fo + 1) * 128)
                for kc in range(8):
                    P.op('pe', lambda e, p_=p_, kc=kc, fs=fs: e.matmul(p_.t[:, 0:n], Wo.t[:, kc, fs], mg.t[:, kc, 0:n], start=(kc == 0), stop=(kc == 7)), [Wo.b, mg.b], [p_.b])
                P.op('dve', lambda e, p_=p_, fo=fo: e.scalar_tensor_tensor(out=XT.t[:, fo, 0:n], in0=p_.t[:, 0:n], scalar=self.modc.t[:, 16 + fo, w:w + 1], in1=XT.t[:, fo, 0:n],
                                                                          op0=ALU.mult, op1=ALU.add), [p_.b, XT.b, self.modc.b], [XT.b])
            P.dma('pool', lambda e, XT=XT, t0=t0, n=n: e.dma_start(out=kp(self.scr['X'])[:, :, t0:t0 + n], in_=XT.t[:, :, 0:n]), [XT.b], [P.D('X', t0)])
            H2 = h2[i % 2]
            self.rms_tile(XT, n, w, self.gm2, 24, pN, sq, rst, tmp, H2, extra_f32=h2f)
            P.dma('pool', lambda e, H2=H2, t0=t0, n=n: e.dma_start(out=kp(self.scr['H2T'])[:, :, t0:t0 + n], in_=H2.t[:, :, 0:n]), [H2.b], [P.D('H2T', t0)])
            if moe:
                for sub in range(n // 128):
                    for kc in range(8):
                        P.op('pe', lambda e, kc=kc, sub=sub: e.matmul(pR.t[:, 0:8], h2f.t[:, kc, sub * 128:(sub + 1) * 128], wr.t[:, kc, :], start=(kc == 0), stop=(kc == 7)),
                             [h2f.b, wr.b], [pR.b])
                    P.op('dve', lambda e: e.tensor_copy(out=lg.t[:], in_=pR.t[:, 0:8]), [pR.b], [lg.b])
                    eq, l2, sel, ex, cmb = (r8[k] for k in ('eq', 'l2', 'sel', 'ex', 'cmb'))
                    mx1, nm1, mx2, ss = (r1[k] for k in ('mx1', 'nm1', 'mx2', 'ss'))
                    P.op('dve', lambda e: e.reduce_max(out=mx1.t[:], in_=lg.t[:], axis=AX.X), [lg.b], [mx1.b])
                    P.op('dve', lambda e: e.tensor_scalar(out=eq.t[:], in0=lg.t[:], scalar1=mx1.t[:, 0:1], scalar2=None, op0=ALU.is_equal), [lg.b, mx1.b], [eq.b])
                    P.op('dve', lambda e: e.scalar_tensor_tensor(out=l2.t[:], in0=eq.t[:], scalar=-1e30, in1=lg.t[:], op0=ALU.mult, op1=ALU.add), [eq.b, lg.b], [l2.b])
                    P.op('dve', lambda e: e.reduce_max(out=mx2.t[:], in_=l2.t[:], axis=AX.X), [l2.b], [mx2.b])
                    P.op('dve', lambda e: e.tensor_scalar(out=sel.t[:], in0=lg.t[:], scalar1=mx2.t[:, 0:1], scalar2=None, op0=ALU.is_ge), [lg.b, mx2.b], [sel.b])
                    P.op('dve', lambda e: e.tensor_scalar(out=nm1.t[:], in0=mx1.t[:], scalar1=-1.0, scalar2=None, op0=ALU.mult), [mx1.b], [nm1.b])
                    P.op('act', lambda e: e.activation(out=ex.t[:], in_=lg.t[:], func=AF.Exp, bias=nm1.t[:, 0:1]), [lg.b, nm1.b], [ex.b])
                    P.op('dve', lambda e: e.tensor_tensor(out=ex.t[:], in0=ex.t[:], in1=sel.t[:], op=ALU.mult), [ex.b, sel.b], [ex.b])
                    P.op('dve', lambda e: e.reduce_sum(out=ss.t[:], in_=ex.t[:], axis=AX.X), [ex.b], [ss.b])
                    P.op('dve', lambda e: e.reciprocal(out=ss.t[:], in_=ss.t[:]), [ss.b], [ss.b])
                    P.op('dve', lambda e: e.tensor_scalar(out=cmb.t[:], in0=ex.t[:], scalar1=ss.t[:, 0:1], scalar2=None, op0=ALU.mult), [ex.b, ss.b], [cmb.b])
                    P.op('pe', lambda e: e.transpose(pRT.t[0:8, 128:256], cmb.t[:], self.C(C_IDN)), [cmb.b, self.cst.b], [pRT.b])
                    ct = cT[sub % 2]
                    P.op('act', lambda e, ct=ct: e.activation(out=ct.t[:], in_=pRT.t[0:8, 128:256], func=AF.Copy), [pRT.b], [ct.b])
                    tt = t0 + sub * 128
                    P.dma('pool', lambda e, ct=ct, tt=tt: e.dma_start(out=self.scr['CMB'][:, tt:tt + 128], in_=ct.t[:]), [ct.b], [P.D('CMB', tt)])
        self.xsrc = self.scr['X']
        self.xsrc_name = 'X'

    def ph_ffn(self, st, l, need_ctx):
        P = self.P
        moe = (l % 2 == 1)
        tiles = self.own_tiles(l, need_ctx)
        NH = DFF // 2
        Wgs = [self.sb(st, 'Wg', [128, 8, NH], BF16) for _ in range(2)]
        Wus = [self.sb(st, 'Wu', [128, 8, NH], BF16) for _ in range(2)]
        Wd = self.sb(st, 'Wd', [128, 11, 1024], BF16)
        h2 = [self.sb(st, 'h2', [128, 8, 512], BF16) for _ in range(1)]
        xT = [self.sb(st, 'xT', [128, 8, 512], F32) for _ in range(1)]
        cw = [self.sb(st, 'cw', [128, 512], F32) for _ in range(2)]
        act = self.sb(st, 'act', [128, 11, 512], BF16)
        sg = [self.sb(st, 'sg', [128, 512], F32) for _ in range(2)]
        tmps = [self.sb(st, 'tmp', [128, 512], F32) for _ in range(2)]
        pG = [self.ps(st, 'pG', [128, 512]) for _ in range(2)]
        pU = [self.ps(st, 'pU', [128, 512]) for _ in range(2)]
        pD = [self.ps(st, 'pD', [128, 512]) for _ in range(2)]
        if moe:
            groups = [(ex, hf) for ex in range(8) for hf in range(2)]
        else:
            groups = [(None, hf) for hf in range(2)]
        def srcs(gi):
            ex, hf = groups[gi]
            if moe:
                return self.inp['moe_gate'][0, ex], self.inp['moe_up'][0, ex], self.inp['moe_down'][0, ex], hf
            return self.inp['ffn_gate'][0], self.inp['ffn_up'][0], self.inp['ffn_down'][0], hf

        def load_gu(gi):
            gsrc, usrc, dsrc, hf = srcs(gi)
            Wg, Wu = Wgs[gi % 2], Wus[gi % 2]
            P.dma('pool', lambda e: e.dma_start(out=Wg.t[:], in_=kp(gsrc)[:, :, hf * NH:(hf + 1) * NH]), [], [Wg.b])
            P.dma('pool', lambda e: e.dma_start(out=Wu.t[:], in_=kp(usrc)[:, :, hf * NH:(hf + 1) * NH]), [], [Wu.b])

        load_gu(0)
        for gi, (ex, hf) in enumerate(groups):
            gsrc, usrc, dsrc, hf = srcs(gi)
            Wg, Wu = Wgs[gi % 2], Wus[gi % 2]
            P.dma('pool', lambda e, dsrc=dsrc, hf=hf: e.dma_start(out=Wd.t[:], in_=kp(dsrc[hf * NH:(hf + 1) * NH, :])), [], [Wd.b])
            if gi + 1 < len(groups):
                load_gu(gi + 1)

            def load_h2(i):
                t0, n, w = tiles[i]
                P.dma('sp', lambda e: e.dma_start(out=h2[0].t[:, :, 0:n], in_=kp(self.scr['H2T'])[:, :, t0:t0 + n]), [P.D('H2T', 0)], [h2[0].b])

            def load_x(i, gi=gi, ex=ex):
                t0, n, w = tiles[i]
                P.dma('sp', lambda e: e.dma_start(out=xT[0].t[:, :, 0:n], in_=kp(self.scr['X'])[:, :, t0:t0 + n]), [P.D('X', t0)], [xT[0].b])
                if moe:
                    c_ = cw[(gi * len(tiles) + i) % 2]
                    P.dma('sp', lambda e: e.dma_start(out=c_.t[:, 0:n], in_=self.scr['CMB'][ex:ex + 1, t0:t0 + n].partition_broadcast(128)), [P.D('CMB', 0)], [c_.b])

            load_h2(0)
            load_x(0)
            for i, (t0, n, w) in enumerate(tiles):
                H2, XT, CW = h2[0], xT[0], cw[(gi * len(tiles) + i) % 2]
                for jf in range(11):
                    g_, u_ = pG[jf % 2], pU[jf % 2]
                    fs = slice(jf * 128, (jf + 1) * 128)
                    for kc in range(8):
                        P.op('pe', lambda e, g_=g_, kc=kc, fs=fs: e.matmul(g_.t[:, 0:n], Wg.t[:, kc, fs], H2.t[:, kc, 0:n], start=(kc == 0), stop=(kc == 7)), [Wg.b, H2.b], [g_.b])
                    for kc in range(8):
                        P.op('pe', lambda e, u_=u_, kc=kc, fs=fs: e.matmul(u_.t[:, 0:n], Wu.t[:, kc, fs], H2.t[:, kc, 0:n], start=(kc == 0), stop=(kc == 7)), [Wu.b, H2.b], [u_.b])
                    s2 = sg[jf % 2]
                    P.op('act', lambda e, g_=g_, s2=s2: e.activation(out=s2.t[:, 0:n], in_=g_.t[:, 0:n], func=AF.Silu), [g_.b], [s2.b])
                    P.op('dve', lambda e, u_=u_, s2=s2, jf=jf: e.tensor_tensor(out=act.t[:, jf, 0:n], in0=u_.t[:, 0:n], in1=s2.t[:, 0:n], op=ALU.mult), [u_.b, s2.b], [act.b])
                if i + 1 < len(tiles):
                    load_h2(i + 1)
                for fo in range(8):
                    d_ = pD[fo % 2]
                    fs = slice(fo * 128, (fo + 1) * 128)
                    for j in range(11):
                        P.op('pe', lambda e, d_=d_, j=j, fs=fs: e.matmul(d_.t[:, 0:n], Wd.t[:, j, fs], act.t[:, j, 0:n], start=(j == 0), stop=(j == 10)), [Wd.b, act.b], [d_.b])
                    gsc = self.modc.t[:, 40 + fo, w:w + 1]
                    if moe:
                        tm = tmps[fo % 2]
                        P.op('act', lambda e, d_=d_, tm=tm, gsc=gsc: e.activation(out=tm.t[:, 0:n], in_=d_.t[:, 0:n], func=AF.Identity, scale=gsc), [d_.b, self.modc.b], [tm.b])
                        P.op('pool', lambda e, tm=tm: e.tensor_tensor(out=tm.t[:, 0:n], in0=tm.t[:, 0:n], in1=CW.t[:, 0:n], op=ALU.mult), [tm.b, CW.b], [tm.b])
                        P.op('dve', lambda e, fo=fo, tm=tm: e.tensor_tensor(out=XT.t[:, fo, 0:n], in0=XT.t[:, fo, 0:n], in1=tm.t[:, 0:n], op=ALU.add), [tm.b, XT.b], [XT.b])
                    else:
                        P.op('dve', lambda e, d_=d_, fo=fo, gsc=gsc: e.scalar_tensor_tensor(out=XT.t[:, fo, 0:n], in0=d_.t[:, 0:n], scalar=gsc, in1=XT.t[:, fo, 0:n], op0=ALU.mult, op1=ALU.add),
                             [d_.b, XT.b, self.modc.b], [XT.b])
                P.dma('pool', lambda e, XT=XT, t0=t0, n=n: e.dma_start(out=kp(self.scr['X'])[:, :, t0:t0 + n], in_=XT.t[:, :, 0:n]), [XT.b], [P.D('X', t0)])
                if i + 1 < len(tiles):
                    load_x(i + 1)

    def final_norm(self):
        P = self.P
        with ExitStack() as st:
            tiles = self.own_tiles(self.nlayers - 1, False)
            xT = [self.sb(st, 'xT', [128, 8, 512], F32) for _ in range(2)]
            oT = [self.sb(st, 'oT', [128, 8, 512], F32) for _ in range(2)]
            sq = self.sb(st, 'sq', [128, 8, 512], BF16)
            rst = self.sb(st, 'rst', [128, 512], F32)
            psb = [self.ps(st, 'psb', [128, 512]) for _ in range(2)]

            def load(i):
                t0, n, w = tiles[i]
                P.dma('sp', lambda e: e.dma_start(out=xT[i % 2].t[:], in_=kp(self.scr['X'])[:, :, t0:t0 + n]), [P.D('X', t0)], [xT[i % 2].b])

            load(0)
            for i, (t0, n, w) in enumerate(tiles):
                if i + 1 < len(tiles):
                    load(i + 1)
                self.rms_tile(xT[i % 2], n, 0, None, 0, psb[i % 2], sq, rst, None, oT[i % 2])
                o = oT[i % 2]
                P.dma('pool', lambda e, o=o, t0=t0, n=n: e.dma_start(out=kp(self.out)[:, :, t0 - LCTX:t0 - LCTX + n], in_=o.t[:]), [o.b], [P.D('out', t0)])
            P.barrier()


def _col(v, n):
    return np.ascontiguousarray(np.asarray(v, np.float32).reshape(n, 128).T)


NA_VAR_ROWS = [0, 1, 2, 3, 4, 64, 122, 123, 124, 125, 126, 127]


def _build_nabias(rpb, flip):
    out = np.full((len(NA_VAR_ROWS), 128, 8, 320), -1e30, np.float32)
    ql = np.arange(64)
    q_true = 63 - ql if flip else ql
    k_true = 63 - ql if flip else ql
    cs_true = np.clip(q_true - 8, 0, 48)
    validc = (k_true[:, None] >= cs_true[None, :]) & (k_true[:, None] < cs_true[None, :] + 16)
    dcol = np.clip(k_true[:, None] - q_true[None, :] + 15, 0, 30)
    for vi, rl in enumerate(NA_VAR_ROWS):
        rs10 = min(max(rl - 4, 0), 118)
        r_true = 127 - rl if flip else rl
        rs_true = min(max(r_true - 4, 0), 120)
        for w in range(10):
            krl = rs10 + w
            kr_true = 127 - krl if flip else krl
            if not (rs_true <= kr_true <= rs_true + 7):
                continue
            drow = kr_true - r_true + 7
            vals = rpb[:, drow][:, dcol]
            vals = np.where(validc[None], vals, np.float32(-1e30))
            j, par = w // 2, w % 2
            out[vi, par * 64:(par + 1) * 64, :, j * 64:(j + 1) * 64] = vals.transpose(1, 0, 2)
    return out.reshape(len(NA_VAR_ROWS), 128, 2560)


def _consts():
    c = np.zeros((128, NCONST), np.float32)
    s = np.arange(128)[:, None]
    l = np.arange(128)[None, :]
    c[:, C_IDN:C_IDN + 128] = (s == l)
    c[:, C_TRIF:C_TRIF + 128] = (s <= l)
    c[:, C_TRIB:C_TRIB + 128] = (s >= l)
    c[:, C_NEGF:C_NEGF + 128] = np.where(l >= s, 0.0, -1e30)
    c[:, C_NEGB:C_NEGB + 128] = np.where(s >= l, 0.0, -1e30)
    c[:, C_ONES:C_ONES + 128] = 1.0
    sel = np.zeros((16, 16, 128), np.float32)
    for e in range(16):
        sel[e, e, :] = 1.0
    return c, sel.reshape(16, 2048)


def _pcol(inp, l, flip):
    p = np.zeros((128, NPC), np.float32)
    p[:, O_ADAB:O_ADAB + 48] = _col(inp['ada_b'][l], 48)
    p[:, O_GMIX:O_GMIX + 8] = _col(inp['norm_mix'][l], 8)
    p[:, O_GFFN:O_GFFN + 8] = _col(inp['norm_ffn'][l], 8)
    scw = np.asarray(inp['ssd_conv_w'][l], np.float32)
    ccw = np.asarray(inp['conf_conv_w'][l], np.float32)
    alog = np.asarray(inp['ssd_a_log'][l], np.float32)
    dtb = np.asarray(inp['ssd_dt_bias'][l], np.float32)
    if flip:
        scw, ccw, alog, dtb = scw[::-1], ccw[::-1], alog[::-1], dtb[::-1]
    p[:, O_SCW:O_SCW + 60] = scw.reshape(5, 12, 128).transpose(2, 1, 0).reshape(128, 60)
    p[:, O_SCB:O_SCB + 12] = _col(inp['ssd_conv_b'][l], 12)
    p[:, O_CCW:O_CCW + 124] = ccw.reshape(31, 4, 128).transpose(2, 1, 0).reshape(128, 124)
    p[:, O_CCB:O_CCB + 4] = _col(inp['conf_conv_b'][l], 4)
    p[:, O_LNG:O_LNG + 4] = _col(inp['conf_ln_g'][l], 4)
    p[:, O_LNB:O_LNB + 4] = _col(inp['conf_ln_b'][l], 4)
    p[:, O_SNORM:O_SNORM + 8] = _col(inp['ssd_norm'][l], 8)
    p[:, O_SD:O_SD + 16] = np.asarray(inp['ssd_d'][l], np.float32)[None, :]
    p[:, O_ALOG:O_ALOG + 32] = alog.reshape(1, 32)
    p[:, O_DTB:O_DTB + 32] = dtb.reshape(1, 32)
    return p


_NC_CACHE = {}


def make_in_maps(inputs, halfmode=True):
    inp = {k: np.asarray(v) for k, v in inputs.items()}
    consts, sel = _consts()
    shared = {}
    for k in ('ada_w', 'ssd_out', 'na_out', 'conf_out', 'w_o', 'ffn_gate', 'ffn_up', 'ffn_down', 'moe_router', 'moe_gate', 'moe_up', 'moe_down'):
        shared[k] = np.ascontiguousarray(inp[k], dtype=np.float32)
    w_in = np.ascontiguousarray(inp['w_in'], dtype=np.float32)
    w_in_f = w_in.copy()
    w_in_f[:, :, 2560:2576] = w_in[:, :, 2576:2592]
    w_in_f[:, :, 2576:2592] = w_in[:, :, 2560:2576]
    per_flip = {}
    for flip in (False, True):
        per_flip[flip] = dict(
            pcol=np.stack([_pcol(inp, 0, flip), _pcol(inp, 1, flip)]),
            nabias=np.stack([_build_nabias(np.asarray(inp['na_rpb'][l], np.float32), flip) for l in range(2)]),
            w_in=w_in_f if flip else w_in)
    maps = []
    for core in range(8):
        b = core % 4
        flip = halfmode and core >= 4
        cx, xx = inp['ctx'][b], inp['x'][b]
        if flip:
            cx, xx = cx[::-1], xx[::-1]
        xin = np.ascontiguousarray(np.concatenate([cx.T, xx.T], axis=1), dtype=np.float32)
        gcol = np.zeros((128, 24), np.float32)
        gcol[:, 0:8] = _col(inp['final_norm'], 8)
        cv = np.stack([_col(inp['c'][b], 8), _col(inp['c_ctx'], 8)], axis=2)
        gcol[:, 8:24] = cv.reshape(128, 16)
        m = dict(shared)
        m.update(per_flip[flip])
        m.update(consts=consts, sel=sel, xin=xin, gcol=gcol)
        maps.append(m)
    return maps


def kernel(**inputs):
    if 'nc' not in _NC_CACHE:
        _NC_CACHE['nc'] = KB().build()
    nc = _NC_CACHE['nc']
    maps = make_in_maps(inputs)
    res = run_bass_kernel_spmd(nc, maps, core_ids=list(range(8)))
    out = np.empty((4, LLAT, D), np.float32)
    for b in range(4):
        out[b, :LLAT // 2] = res.results[b]['outT'].T
        out[b, LLAT // 2:] = res.results[b + 4]['outT'].T[::-1]
    return out
```
